# Optimizing a Trainium2 kernel written in Bass

```python
import jax, jax.numpy as jnp
from jax import lax
import numpy as np

D_MODEL = 2048
BATCH = 8
SEQ = 2048
DEPTH = 1

MEM_LEN = 256
POOL_WINDOWS = (2, 4, 8, 16)
N_POOL_GROUPS = 4
POOL_GROUP_DIM = D_MODEL // 8
POOL_WIDTH = N_POOL_GROUPS * POOL_GROUP_DIM
N_FOURIER_GROUPS = 4
FOURIER_GROUP_DIM = D_MODEL // 16
FOURIER_WIDTH = N_FOURIER_GROUPS * FOURIER_GROUP_DIM
N_MEM_HEADS = 4
MEM_HEAD_DIM = D_MODEL // 16
MEM_WIDTH = N_MEM_HEADS * MEM_HEAD_DIM
MIX_WIDTH = POOL_WIDTH + FOURIER_WIDTH + MEM_WIDTH
N_BRANCHES = 3
IN_WIDTH = MIX_WIDTH + N_BRANCHES * D_MODEL
N_EXPERTS = 16
CAPACITY_FACTOR = 2
D_EXPERT = D_MODEL
EPS = 1e-6

kernel_name = "hybrid_pool_fourier_memattn_ecmoe_block"


def _rmsnorm(x, g):
    x32 = x.astype(jnp.float32)
    inv = lax.rsqrt(jnp.mean(x32 * x32, axis=-1, keepdims=True) + EPS)
    return (x32 * inv).astype(x.dtype) * g


def _centred_mean_minus_self(u, window):
    S = u.shape[1]
    u32 = u.astype(jnp.float32)
    csum = jnp.concatenate([jnp.zeros_like(u32[:, :1]), jnp.cumsum(u32, axis=1)], axis=1)
    pos = jnp.arange(S)
    lo = jnp.clip(pos - window // 2, 0, S)
    hi = jnp.clip(pos + window - window // 2, 0, S)
    total = jnp.take(csum, hi, axis=1) - jnp.take(csum, lo, axis=1)
    count = (hi - lo).astype(jnp.float32)[None, :, None]
    return (total / count - u32).astype(u.dtype)


def _fourier_2d_real(u):
    spec = jnp.fft.fft2(u.astype(jnp.float32), axes=(1, 3), norm="ortho")
    return spec.real.astype(u.dtype)


def _mixing_sublayer(x, mem, norm_mix_g, norm_mem_g, w_in, pool_w, pool_scale, fourier_w,
                     w_kv_mem, proj_pool, proj_fourier, proj_mem, w_out):
    B, S, D = x.shape
    h = _rmsnorm(x, norm_mix_g)
    z = h @ w_in
    u_pool = z[..., :POOL_WIDTH]
    u_four = z[..., POOL_WIDTH:POOL_WIDTH + FOURIER_WIDTH]
    q = z[..., POOL_WIDTH + FOURIER_WIDTH:MIX_WIDTH]
    gate_pre = z[..., MIX_WIDTH:]

    u_pool = u_pool.reshape(B, S, N_POOL_GROUPS, POOL_GROUP_DIM)
    pooled = jnp.stack([_centred_mean_minus_self(u_pool[:, :, g], w)
                        for g, w in enumerate(POOL_WINDOWS)], axis=2)
    y_a = jnp.einsum('bsgc,gcd->bsgd', pooled, pool_w).reshape(B, S, POOL_WIDTH) * pool_scale
    y_a = y_a @ proj_pool

    u_four = u_four.reshape(B, S, N_FOURIER_GROUPS, FOURIER_GROUP_DIM)
    f = _fourier_2d_real(u_four)
    y_b = jnp.einsum('bsgc,gcd->bsgd', f, fourier_w).reshape(B, S, FOURIER_WIDTH)
    y_b = y_b @ proj_fourier

    mem_n = _rmsnorm(mem, norm_mem_g)
    kv = mem_n @ w_kv_mem
    M = mem.shape[1]
    k = kv[..., :MEM_WIDTH].reshape(B, M, N_MEM_HEADS, MEM_HEAD_DIM)
    v = kv[..., MEM_WIDTH:].reshape(B, M, N_MEM_HEADS, MEM_HEAD_DIM)
    qh = q.reshape(B, S, N_MEM_HEADS, MEM_HEAD_DIM)
    scores = jnp.einsum('bshd,bmhd->bhsm', qh, k).astype(jnp.float32) * (MEM_HEAD_DIM ** -0.5)
    probs = jax.nn.softmax(scores, axis=-1).astype(v.dtype)
    o = jnp.einsum('bhsm,bmhd->bshd', probs, v).reshape(B, S, MEM_WIDTH)
    y_c = o @ proj_mem

    gates = jax.nn.sigmoid(gate_pre.astype(jnp.float32)).astype(x.dtype).reshape(B, S, N_BRANCHES, D)
    merged = gates[:, :, 0] * y_a + gates[:, :, 1] * y_b + gates[:, :, 2] * y_c
    return merged @ w_out


def _expert_choice_moe(h, w_router, w_expert_gate, w_expert_up, w_expert_down):
    B, S, D = h.shape
    affinity = jax.nn.softmax((h @ w_router).astype(jnp.float32), axis=-1)
    capacity = CAPACITY_FACTOR * S // N_EXPERTS
    top_aff, top_idx = lax.top_k(jnp.swapaxes(affinity, 1, 2), capacity)
    xin = jax.vmap(lambda hb, ib: hb[ib])(h, top_idx)
    hidden = jax.nn.silu(jnp.einsum('becd,edf->becf', xin, w_expert_gate)) * \
        jnp.einsum('becd,edf->becf', xin, w_expert_up)
    y = jnp.einsum('becf,efd->becd', hidden, w_expert_down) * top_aff[..., None].astype(h.dtype)
    return jax.vmap(lambda yb, ib: jnp.zeros((S, D), yb.dtype).at[ib.reshape(-1)].add(yb.reshape(-1, D)))(y, top_idx)


def setup_inputs(seed: int = 0) -> dict:
    key = jax.random.key(seed)
    ks = jax.random.split(key, 20)
    L, D, E, F = DEPTH, D_MODEL, N_EXPERTS, D_EXPERT
    nrm = lambda k, shape, fan_in: jax.random.normal(k, shape, jnp.float32) * (fan_in ** -0.5)
    gain = lambda k, shape: 1.0 + 0.02 * jax.random.normal(k, shape, jnp.float32)
    return {
        "x": jax.random.normal(ks[0], (BATCH, SEQ, D), jnp.float32),
        "mem": jax.random.normal(ks[1], (BATCH, MEM_LEN, D), jnp.float32),
        "norm_mix_g": gain(ks[2], (L, D)),
        "norm_mem_g": gain(ks[3], (L, D)),
        "w_in": nrm(ks[4], (L, D, IN_WIDTH), D),
        "pool_w": nrm(ks[5], (L, N_POOL_GROUPS, POOL_GROUP_DIM, POOL_GROUP_DIM), POOL_GROUP_DIM),
        "pool_scale": gain(ks[6], (L, POOL_WIDTH)),
        "fourier_w": nrm(ks[7], (L, N_FOURIER_GROUPS, FOURIER_GROUP_DIM, FOURIER_GROUP_DIM), FOURIER_GROUP_DIM),
        "w_kv_mem": nrm(ks[8], (L, D, 2 * MEM_WIDTH), D),
        "proj_pool": nrm(ks[9], (L, POOL_WIDTH, D), POOL_WIDTH),
        "proj_fourier": nrm(ks[10], (L, FOURIER_WIDTH, D), FOURIER_WIDTH),
        "proj_mem": nrm(ks[11], (L, MEM_WIDTH, D), MEM_WIDTH),
        "w_out": nrm(ks[12], (L, D, D), D),
        "norm_ffn_g": gain(ks[13], (L, D)),
        "w_router": nrm(ks[14], (L, D, E), D),
        "w_expert_gate": nrm(ks[15], (L, E, D, F), D),
        "w_expert_up": nrm(ks[16], (L, E, D, F), D),
        "w_expert_down": nrm(ks[17], (L, E, F, D), F),
        "norm_final_g": gain(ks[18], (D,)),
    }


def reference(x, mem, norm_mix_g, norm_mem_g, w_in, pool_w, pool_scale, fourier_w, w_kv_mem,
              proj_pool, proj_fourier, proj_mem, w_out, norm_ffn_g, w_router, w_expert_gate,
              w_expert_up, w_expert_down, norm_final_g):
    for l in range(DEPTH):
        x = x + _mixing_sublayer(x, mem, norm_mix_g[l], norm_mem_g[l], w_in[l], pool_w[l],
                                 pool_scale[l], fourier_w[l], w_kv_mem[l], proj_pool[l],
                                 proj_fourier[l], proj_mem[l], w_out[l])
        h2 = _rmsnorm(x, norm_ffn_g[l])
        x = x + _expert_choice_moe(h2, w_router[l], w_expert_gate[l], w_expert_up[l], w_expert_down[l])
    return _rmsnorm(x, norm_final_g)
```

```python
import numpy as np
import concourse.bass as bass
import concourse.mybir as mybir
from concourse.bass_utils import run_bass_kernel_spmd

F32 = mybir.dt.float32
BF16 = mybir.dt.bfloat16
AF = mybir.ActivationFunctionType
ALU = mybir.AluOpType
AX = mybir.AxisListType

SAME_ENGINE_SYNC = True
S = 2048
D = 2048
E = 16
CAP = 256
EPS = 1e-6
BASE = 16640


class Op:
    __slots__ = ("eng", "fn", "is_dma", "chan", "needs_inc", "deps", "cnt")

    def __init__(self, eng, fn, is_dma=False, chan=None):
        self.eng = eng
        self.fn = fn
        self.is_dma = is_dma
        self.chan = chan
        self.needs_inc = is_dma
        self.deps = []
        self.cnt = None


class Plan:
    ENGS = ("pe", "act", "dve", "pool", "sp")

    def __init__(self, nc):
        self.nc = nc
        self.ops = {e: [] for e in self.ENGS}
        self.last_w = {}
        self.readers = {}
        self.chan_last = {}
        self.chan_count = {}

    def _add(self, op, reads, writes):
        deps = {}
        for r in reads:
            w = self.last_w.get(r)
            if w is not None:
                deps[id(w)] = w
        for k in writes:
            w = self.last_w.get(k)
            if w is not None:
                deps[id(w)] = w
            for rd in self.readers.get(k, ()):
                deps[id(rd)] = rd
        for r in reads:
            self.readers.setdefault(r, []).append(op)
        for k in writes:
            self.last_w[k] = op
            self.readers[k] = []
        deps.pop(id(op), None)
        for d in deps.values():
            if d.is_dma:
                op.deps.append(d)
            elif d.eng == op.eng and not op.is_dma:
                if d.eng != "pe" and SAME_ENGINE_SYNC:
                    d.needs_inc = True
                    op.deps.append(d)
            else:
                d.needs_inc = True
                op.deps.append(d)
        self.ops[op.eng].append(op)
        return op

    def alias(self, new_keys, dead_keys):
        ops = {}
        for k in dead_keys:
            w = self.last_w.get(k)
            if w is not None:
                ops[id(w)] = w
            for r in self.readers.get(k, ()):
                ops[id(r)] = r
        for nk in new_keys:
            lst = self.readers.setdefault(nk, [])
            have = {id(o) for o in lst}
            for o in ops.values():
                if id(o) not in have:
                    lst.append(o)

    def op(self, eng, fn, reads=(), writes=()):
        return self._add(Op(eng, fn), reads, writes)

    def dma(self, queue, chan, fn, reads=(), writes=()):
        op = Op(queue, fn, is_dma=True, chan=chan)
        prev = self.chan_last.get(chan)
        if prev is not None:
            op.deps.append(prev)
        self.chan_last[chan] = op
        c = self.chan_count.get(chan, 0) + 1
        self.chan_count[chan] = c
        op.cnt = 16 * c
        return self._add(op, reads, writes)

    def emit(self, final_waits=()):
        nc = self.nc
        for e in self.ENGS:
            c = 0
            for op in self.ops[e]:
                if op.is_dma:
                    continue
                if op.needs_inc:
                    c += 1
                    op.cnt = c
        sems = {}

        def sem_of(key):
            if key not in sems:
                sems[key] = nc.alloc_semaphore("s_%d" % len(sems))
            return sems[key]

        def key_of(d):
            return ("ch", d.chan) if d.is_dma else ("eng", d.eng)

        plan = self

        def run(engname, eng):
            waited = {}
            for op in plan.ops[engname]:
                need = {}
                for d in op.deps:
                    k = key_of(d)
                    if d.cnt > need.get(k, 0):
                        need[k] = d.cnt
                for k, v in need.items():
                    if waited.get(k, 0) >= v:
                        continue
                    eng.wait_ge(sem_of(k), v)
                    waited[k] = v
                ins = op.fn(eng)
                if op.is_dma:
                    ins.then_inc(sem_of(("ch", op.chan)), 16)
                elif op.needs_inc:
                    ins.then_inc(sem_of(("eng", engname)), 1)
            if engname == "sp":
                for d in final_waits:
                    eng.wait_ge(sem_of(key_of(d)), d.cnt)

        with nc.Block() as block:
            @block.tensor
            def _(eng):
                run("pe", eng)

            @block.scalar
            def _(eng):
                run("act", eng)

            @block.vector
            def _(eng):
                run("dve", eng)

            @block.gpsimd
            def _(eng):
                run("pool", eng)

            @block.sync
            def _(eng):
                run("sp", eng)


def build(stage=99, dbg=None):
    nc = bass.Bass("TRN2", target_bir_lowering=False)
    P = Plan(nc)
    in_shapes = {}

    def inp(name, shape):
        in_shapes[name] = tuple(shape)
        return nc.dram_tensor(name, list(shape), F32, kind="ExternalInput").ap()

    def sb(name, shape, dt, kib):
        return nc.alloc_sbuf_tensor_at(name, list(shape), dt, offset=BASE + int(round(kib * 1024)))

    psA = nc.alloc_psum_tensor("psA", [128, 2048], F32)
    psB = nc.alloc_psum_tensor("psB", [128, 2048], F32)

    def bank(i):
        t = psA if i < 4 else psB
        j = i % 4
        return t[:, j * 512:(j + 1) * 512]

    psA_bf = psA.bitcast(BF16)
    psB_bf = psB.bitcast(BF16)

    def bank_bf(i):
        t = psA_bf if i < 4 else psB_bf
        j = i % 4
        return t[:, j * 1024:(j + 1) * 1024]

    bank_ctr = [0]

    def next_bank():
        b = bank_ctr[0] % 8
        bank_ctr[0] += 1
        return b

    cp_ctr = [0]

    def evac(out_ap, in_ap, reads, writes, scale=None):
        cp_ctr[0] += 1
        if scale is not None:
            return P.op("act", lambda e: e.activation(out=out_ap, in_=in_ap, func=AF.Identity, scale=scale),
                        reads=reads, writes=writes)
        if cp_ctr[0] % 2 == 0:
            return P.op("act", lambda e: e.copy(out=out_ap, in_=in_ap), reads=reads, writes=writes)
        return P.op("dve", lambda e: e.tensor_copy(out=out_ap, in_=in_ap), reads=reads, writes=writes)

    def mm(out_ap, lhsT, rhs, start, stop, reads, writes):
        return P.op("pe", lambda e: e.matmul(out_ap, lhsT, rhs, start=start, stop=stop), reads=reads, writes=writes)

    wslot = [sb("wslot%d" % i, [128, 16, 256], BF16, 64 + 8 * i) for i in range(4)]
    slot_ctr = [0]

    def next_slot():
        s = slot_ctr[0] % 4
        slot_ctr[0] += 1
        return s

    CK = 96.0
    ones_bf = sb("ones_bf", [128, 128], BF16, CK)
    ident_bf = sb("ident_bf", [128, 128], BF16, CK + 0.25)
    tri_bf = sb("tri_bf", [128, 128], BF16, CK + 0.5)
    c128_bf = sb("c128_bf", [128, 128], BF16, CK + 0.75)
    ns128_bf = sb("ns128_bf", [128, 128], BF16, CK + 1.0)
    ident_f = sb("ident_f", [128, 128], F32, CK + 1.25)
    iota_f = sb("iota_f", [128, 256], F32, CK + 1.75)
    gmix = sb("gmix", [128, 16], F32, CK + 2.75)
    gmem = sb("gmem", [128, 16], F32, CK + 2.8125)
    pscale = sb("pscale", [128, 8], F32, CK + 2.875)
    wr_bf = sb("wr_bf", [128, 16, 16], BF16, CK + 3.0)
    aff_tm = sb("aff_tm", [128, 16, 16], F32, CK + 3.5)
    mask_tm = sb("mask_tm", [128, 16, 16], F32, CK + 4.5)
    pos_tm = sb("pos_tm", [128, 16, 16], F32, CK + 5.5)
    A_tm = sb("A_tm", [128, 16, 16], F32, CK + 6.5)
    small = sb("small", [128, 64], F32, CK + 7.5)
    maskbf_tm = sb("maskbf_tm", [128, 16, 16], BF16, CK + 7.75)

    consts = inp("cst_mats", [6, 128, 128])
    iota_d = inp("cst_iota", [128, 256])
    vec_d = inp("cst_vecs", [128, 40])

    for i, t in enumerate((ones_bf, ident_bf, tri_bf, c128_bf, ns128_bf)):
        P.dma("pool", "cst%d" % i, lambda e, t=t, i=i: e.dma_start(out=t[:], in_=consts[i]), writes=(t.name,))
    P.dma("sp", "cstf", lambda e: e.dma_start(out=ident_f[:], in_=consts[1]), writes=("ident_f",))
    P.dma("sp", "csti", lambda e: e.dma_start(out=iota_f[:], in_=iota_d), writes=("iota_f",))
    P.dma("sp", "cstv0", lambda e: e.dma_start(out=gmix[:], in_=vec_d[:, 0:16]), writes=("gmix",))
    P.dma("sp", "cstv1", lambda e: e.dma_start(out=gmem[:], in_=vec_d[:, 16:32]), writes=("gmem",))
    P.dma("sp", "cstv2", lambda e: e.dma_start(out=pscale[:], in_=vec_d[:, 32:40]), writes=("pscale",))

    dbg_out = {}
    final_ops = []

    def dump(name, tensor_ap, shape, dt, reads):
        o = nc.dram_tensor("dbg_" + name, list(shape), dt, kind="ExternalOutput").ap()
        dbg_out[name] = True
        final_ops.append(P.dma("sp", "dbg_" + name, lambda e: e.dma_start(out=o, in_=tensor_ap), reads=reads))

    def finish():
        P.emit(final_waits=final_ops)
        return nc, in_shapes

    kT = sb("kT", [128, 4, 256], BF16, 104.5)
    vv = sb("vv", [128, 2, 512], BF16, 106.5)
    T0 = 172.5
    memst = sb("memst", [128, 16, 256], F32, T0)
    memsq = sb("memsq", [128, 16, 256], BF16, T0 + 16)
    invBm = sb("invBm", [128, 256], F32, T0 + 24)
    memnT = sb("memnT", [128, 16, 256], BF16, T0 + 25)

    memT_d = inp("memT", [2048, 256]).rearrange("(c p) m -> p c m", p=128)
    wkv_d = inp("w_kv", [2048, 1024]).rearrange("(c p) n -> p c n", p=128)

    P.dma("sp", "memst", lambda e: e.dma_start(out=memst[:], in_=memT_d), writes=("memst",))
    P.op("act", lambda e: e.activation(out=memsq[:], in_=memst[:], func=AF.Square), reads=("memst",), writes=("memsq",))
    b = next_bank()
    for c in range(16):
        mm(bank(b)[:, 0:256], ones_bf[:], memsq[:, c, :], c == 0, c == 15,
           reads=("memsq", "ones_bf"), writes=(("ps", b),))
    P.op("act", lambda e, b=b: e.activation(out=invBm[:], in_=bank(b)[:, 0:256], func=AF.Sqrt, bias=EPS, scale=1.0 / D),
         reads=(("ps", b),), writes=("invBm",))
    P.op("dve", lambda e: e.reciprocal(out=invBm[:], in_=invBm[:]), reads=("invBm",), writes=("invBm",))
    for c in range(16):
        P.op("dve", lambda e, c=c: e.scalar_tensor_tensor(out=memnT[:, c, :], in0=memst[:, c, :], scalar=gmem[:, c:c + 1],
                                                         in1=invBm[:], op0=ALU.mult, op1=ALU.mult),
             reads=("memst", "invBm", "gmem"), writes=(("memnT", c),))
    memnT_all = tuple(("memnT", c) for c in range(16))
    for blk in range(4):
        s = next_slot()
        P.dma("pool", "ws%d" % s, lambda e, s=s, blk=blk: e.dma_start(out=wslot[s][:], in_=wkv_d[:, :, blk * 256:(blk + 1) * 256]),
              writes=(("ws", s),))
        if blk < 2:
            for hh in range(2):
                h = 2 * blk + hh
                b = next_bank()
                for c in range(16):
                    mm(bank(b)[:, 0:256], wslot[s][:, c, hh * 128:(hh + 1) * 128], memnT[:, c, :], c == 0, c == 15,
                       reads=(("ws", s),) + memnT_all, writes=(("ps", b),))
                evac(kT[:, h, :], bank(b)[:, 0:256], reads=(("ps", b),), writes=("kT",))
        else:
            for mc in range(2):
                b = next_bank()
                for c in range(16):
                    mm(bank(b)[:, 0:256], memnT[:, c, mc * 128:(mc + 1) * 128], wslot[s][:, c, :], c == 0, c == 15,
                       reads=(("ws", s),) + memnT_all, writes=(("ps", b),))
                evac(vv[:, mc, (blk - 2) * 256:(blk - 1) * 256], bank(b)[:, 0:256], reads=(("ps", b),), writes=("vv",))
    if stage == 0:
        dump("kT", kT[:], [128, 4, 256], BF16, ("kT",))
        dump("vv", vv[:], [128, 2, 512], BF16, ("vv",))
        return finish()

    hT = sb("hT", [128, 16, 2048], BF16, 0)
    xst = [sb("xst%d" % i, [128, 2048], F32, T0 + 8 * i) for i in range(2)]
    sq = [sb("sq%d" % i, [128, 2048], BF16, T0 + 16 + 4 * i) for i in range(2)]
    invB = sb("invB", [128, 2048], F32, T0 + 24)
    xT_d = inp("xT", [2048, 2048]).rearrange("(c p) t -> p c t", p=128)

    P.alias([("xst", 0), ("xst", 1), ("sq", 0), ("sq", 1), "invB"], memnT_all + ("memst", "memsq", "invBm"))
    bks = [next_bank() for _ in range(4)]
    for c in range(16):
        i = c % 2
        P.dma("sp", "xst%d" % i, lambda e, i=i, c=c: e.dma_start(out=xst[i][:], in_=xT_d[:, c, :]),
              writes=(("xst", i),))
        P.op("act", lambda e, i=i: e.activation(out=sq[i][:], in_=xst[i][:], func=AF.Square),
             reads=(("xst", i),), writes=(("sq", i),))
        for tb in range(4):
            mm(bank(bks[tb]), ones_bf[:], sq[i][:, tb * 512:(tb + 1) * 512], c == 0, c == 15,
               reads=(("sq", i), "ones_bf"), writes=(("ps", bks[tb]),))
    for tb in range(4):
        P.op("act", lambda e, tb=tb, bks=bks: e.activation(out=invB[:, tb * 512:(tb + 1) * 512], in_=bank(bks[tb]), func=AF.Sqrt,
                                                 bias=EPS, scale=1.0 / D),
             reads=(("ps", bks[tb]),), writes=("invB",))
    P.op("dve", lambda e: e.reciprocal(out=invB[:], in_=invB[:]), reads=("invB",), writes=("invB",))
    for c in range(16):
        i = c % 2
        P.dma("sp", "xst%d" % i, lambda e, i=i, c=c: e.dma_start(out=xst[i][:], in_=xT_d[:, c, :]), writes=(("xst", i),))
        P.op("dve", lambda e, i=i, c=c: e.scalar_tensor_tensor(out=hT[:, c, :], in0=xst[i][:], scalar=gmix[:, c:c + 1],
                                                              in1=invB[:], op0=ALU.mult, op1=ALU.mult),
             reads=(("xst", i), "invB", "gmix"), writes=(("hT", c),))
    hT_all = tuple(("hT", c) for c in range(16))
    if stage == 1:
        dump("hT", hT[:], [128, 16, 2048], BF16, hT_all)
        return finish()

    zf = sb("zf", [128, 16, 512], BF16, 108.5)
    qT = sb("qT", [128, 4, 2048], BF16, 124.5)
    yapT = sb("yapT", [128, 8, 2048], BF16, 140.5)
    pooled = sb("pooled", [128, 2, 2048], BF16, 108.5)
    LP = 2048 + 32
    upad = sb("upad", [128, LP], F32, T0)
    tA = sb("tA", [128, LP], F32, T0 + 8.25)
    tB = sb("tB", [128, LP], F32, T0 + 16.5)
    invc = sb("invc", [128, 2048], F32, T0 + 24.75)
    pw_sb = sb("pw_sb", [128, 2, 256], BF16, 205.5)
    win_d = inp("w_in", [2048, 8192]).rearrange("(c p) n -> p c n", p=128)
    invc_d = inp("cst_invc", [4, 128, 2048])
    poolw_d = inp("pool_w", [1024, 256]).rearrange("(g k p) n -> g p k n", k=2, p=128)

    p0dead = (("xst", 0), ("xst", 1), ("sq", 0), ("sq", 1), "invB")
    P.alias(["upad", "tA", "tB", "invc", "pw_sb"], p0dead)
    P.op("pool", lambda e: e.memset(upad[:], 0.0), reads=(), writes=("upad",))
    P.op("pool", lambda e: e.memset(tA[:], 0.0), reads=(), writes=("tA",))
    P.op("pool", lambda e: e.memset(tB[:], 0.0), reads=(), writes=("tB",))

    for cb in range(8):
        s = next_slot()
        P.dma("pool", "ws%d" % s, lambda e, s=s, cb=cb: e.dma_start(out=wslot[s][:], in_=win_d[:, :, cb * 256:(cb + 1) * 256]),
              writes=(("ws", s),))
        if cb < 4:
            g = cb
            P.dma("sp", "invc", lambda e, g=g: e.dma_start(out=invc[:], in_=invc_d[g]), writes=("invc",))
            P.dma("pool", "pw", lambda e, g=g: e.dma_start(out=pw_sb[:], in_=poolw_d[g]), writes=("pw_sb",))
            for pc in range(2):
                for tb in range(4):
                    b = next_bank()
                    for c in range(16):
                        mm(bank(b), wslot[s][:, c, pc * 128:(pc + 1) * 128], hT[:, c, tb * 512:(tb + 1) * 512], c == 0, c == 15,
                           reads=(("ws", s), ("hT", c)), writes=(("ps", b),))
                    P.op("act", lambda e, b=b, tb=tb: e.copy(out=upad[:, 16 + tb * 512:16 + (tb + 1) * 512], in_=bank(b)),
                         reads=(("ps", b),), writes=("upad",))
                lvl = g + 1
                src = upad
                bufs = [tA, tB]
                sh = [(1, 0), (1, -1), (2, -2), (4, -4)]
                specs = [(1, 0, 1, LP), (1, 1, 2, LP - 1), (2, 2, 4, LP - 3), (4, 4, 8, LP - 7)]
                cur = upad
                curname = "upad"
                for l in range(lvl):
                    dl, dr, lo, hi = specs[l]
                    dst = bufs[l % 2]
                    dname = "tA" if l % 2 == 0 else "tB"
                    P.op("dve", lambda e, dst=dst, cur=cur, dl=dl, dr=dr, lo=lo, hi=hi:
                         e.tensor_tensor(out=dst[:, lo:hi], in0=cur[:, lo - dl:hi - dl], in1=cur[:, lo + dr:hi + dr], op=ALU.add),
                         reads=(curname,), writes=(dname,))
                    cur, curname = dst, dname
                oth = bufs[lvl % 2]
                oname = "tA" if lvl % 2 == 0 else "tB"
                P.op("dve", lambda e, cur=cur, oth=oth: e.tensor_tensor(out=oth[:, 16:16 + 2048], in0=cur[:, 16:16 + 2048],
                                                                       in1=invc[:], op=ALU.mult),
                     reads=(curname, "invc"), writes=(oname,))
                P.op("dve", lambda e, oth=oth, pc=pc: e.tensor_tensor(out=pooled[:, pc, :], in0=oth[:, 16:16 + 2048],
                                                                     in1=upad[:, 16:16 + 2048], op=ALU.subtract),
                     reads=(oname, "upad"), writes=(("pooled", pc),))
            for oc in range(2):
                for tb in range(4):
                    b = next_bank()
                    for kc in range(2):
                        mm(bank(b), pw_sb[:, kc, oc * 128:(oc + 1) * 128], pooled[:, kc, tb * 512:(tb + 1) * 512], kc == 0, kc == 1,
                           reads=("pw_sb", ("pooled", 0), ("pooled", 1)), writes=(("ps", b),))
                    ch = 2 * g + oc
                    evac(yapT[:, ch, tb * 512:(tb + 1) * 512], bank(b), reads=(("ps", b), "pscale"), writes=(("yapT", ch),),
                         scale=pscale[:, ch:ch + 1])
        elif cb < 6:
            if cb == 4:
                P.alias([("zf", t) for t in range(16)], [("pooled", 0), ("pooled", 1)])
            for tt in range(16):
                if tt % 2 == 0:
                    b = next_bank()
                half = (tt % 2) * 256
                for c in range(16):
                    mm(bank(b)[:, half:half + 256], hT[:, c, tt * 128:(tt + 1) * 128], wslot[s][:, c, :], c == 0, c == 15,
                       reads=(("ws", s), ("hT", c)), writes=(("ps", b),))
                evac(zf[:, tt, (cb - 4) * 256:(cb - 3) * 256], bank(b)[:, half:half + 256], reads=(("ps", b),),
                     writes=(("zf", tt),))
        else:
            for hh in range(2):
                h = 2 * (cb - 6) + hh
                for tb in range(4):
                    b = next_bank()
                    for c in range(16):
                        mm(bank(b), wslot[s][:, c, hh * 128:(hh + 1) * 128], hT[:, c, tb * 512:(tb + 1) * 512], c == 0, c == 15,
                           reads=(("ws", s), ("hT", c)), writes=(("ps", b),))
                    evac(qT[:, h, tb * 512:(tb + 1) * 512], bank(b), reads=(("ps", b),), writes=(("qT", h),))
    yapT_all = tuple(("yapT", c) for c in range(8))
    zf_all = tuple(("zf", t) for t in range(16))
    if stage == 2:
        dump("yapT", yapT[:], [128, 8, 2048], BF16, yapT_all)
        dump("zf", zf[:], [128, 16, 512], BF16, zf_all)
        dump("qT", qT[:], [128, 4, 2048], BF16, tuple(("qT", h) for h in range(4)))
        return finish()

    ocT = sb("ocT", [128, 4, 2048], BF16, T0)
    expT = [sb("expT%d" % i, [128, 2, 512], BF16, T0 + 16 + 2 * i) for i in range(2)]
    rden = [sb("rden%d" % i, [128, 512], F32, T0 + 20 + 2 * i) for i in range(2)]
    pooldead = ("upad", "tA", "tB", "invc")
    P.alias([("expT", 0), ("expT", 1), ("rden", 0), ("rden", 1)] + [("ocT", h) for h in range(4)], pooldead)
    it = 0
    for h in range(4):
        for tb in range(4):
            i = it % 2
            it += 1
            for mc in range(2):
                b = next_bank()
                mm(bank(b), kT[:, h, mc * 128:(mc + 1) * 128], qT[:, h, tb * 512:(tb + 1) * 512], True, True,
                   reads=("kT", ("qT", h)), writes=(("ps", b),))
                P.op("act", lambda e, b=b, i=i, mc=mc: e.activation(out=expT[i][:, mc, :], in_=bank(b), func=AF.Exp,
                                                                   scale=float(128 ** -0.5)),
                     reads=(("ps", b),), writes=(("expT", i),))
            bd = next_bank()
            for mc in range(2):
                mm(bank(bd), ones_bf[:], expT[i][:, mc, :], mc == 0, mc == 1, reads=(("expT", i), "ones_bf"), writes=(("ps", bd),))
            bo = next_bank()
            for mc in range(2):
                mm(bank(bo), vv[:, mc, h * 128:(h + 1) * 128], expT[i][:, mc, :], mc == 0, mc == 1,
                   reads=(("expT", i), "vv"), writes=(("ps", bo),))
            P.op("dve", lambda e, bd=bd, i=i: e.reciprocal(out=rden[i][:], in_=bank(bd)), reads=(("ps", bd),),
                 writes=(("rden", i),))
            P.op("dve", lambda e, bo=bo, i=i, h=h, tb=tb: e.tensor_tensor(out=ocT[:, h, tb * 512:(tb + 1) * 512], in0=bank(bo),
                                                                         in1=rden[i][:], op=ALU.mult),
                 reads=(("ps", bo), ("rden", i)), writes=(("ocT", h),))
    ocT_all = tuple(("ocT", h) for h in range(4))
    if stage == 3:
        dump("ocT", ocT[:], [128, 4, 2048], BF16, ocT_all)
        return finish()

    ybT = sb("ybT", [128, 4, 2048], BF16, T0 + 16)
    fw_sb = sb("fw_sb", [128, 4, 128], BF16, 124.5)
    Wc = sb("Wc", [128, 4, 128], BF16, 125.5)
    Wsn = sb("Wsn", [128, 4, 128], BF16, 126.5)
    T1sb = [sb("T1sb%d" % i, [128, 512], BF16, 127.5 + i) for i in range(4)]
    T2sb = [sb("T2sb%d" % i, [128, 512], BF16, 131.5 + i) for i in range(4)]
    qdead = tuple(("qT", h) for h in range(4))
    attdead = (("expT", 0), ("expT", 1), ("rden", 0), ("rden", 1))
    fw_d = inp("fourier_w", [512, 128]).rearrange("(g p) n -> p g n", p=128)
    cosS_d = inp("cst_cosS", [2048, 2048]).rearrange("(c p) k -> p c k", p=128)
    sinS_d = inp("cst_sinS", [2048, 2048]).rearrange("(c p) k -> p c k", p=128)
    P.alias(["fw_sb", "Wc", "Wsn"] + [("T1sb", i) for i in range(4)] + [("T2sb", i) for i in range(4)], qdead)
    P.alias([("ybT", g) for g in range(4)], attdead)
    P.dma("pool", "fw", lambda e: e.dma_start(out=fw_sb[:], in_=fw_d), writes=("fw_sb",))
    for g in range(4):
        for (cm, dstt, nm) in ((c128_bf, Wc, "Wc"), (ns128_bf, Wsn, "Wsn")):
            b = next_bank()
            mm(bank(b)[:, 0:128], cm[:], fw_sb[:, g, :], True, True, reads=("fw_sb", cm.name), writes=(("ps", b),))
            evac(dstt[:, g, :], bank(b)[:, 0:128], reads=(("ps", b),), writes=(nm,))
    wslot8 = [sb("wslot8_%d" % i, [128, 8, 512], BF16, 64 + 8 * i) for i in range(4)]
    for kb in range(4):
        for (nm, src, dst, dn) in (("c", cosS_d, T1sb, "T1sb"), ("s", sinS_d, T2sb, "T2sb")):
            sl = []
            for hf in range(2):
                s = next_slot()
                sl.append(s)
                P.dma("pool", "ws%d" % s, lambda e, s=s, src=src, hf=hf, kb=kb: e.dma_start(
                    out=wslot8[s][:], in_=src[:, hf * 8:(hf + 1) * 8, kb * 512:(kb + 1) * 512]), writes=(("ws", s),))
            for g in range(4):
                b = next_bank()
                for sc in range(16):
                    s = sl[sc // 8]
                    mm(bank(b), zf[:, sc, g * 128:(g + 1) * 128], wslot8[s][:, sc % 8, :], sc == 0, sc == 15,
                       reads=(("ws", s), ("zf", sc)), writes=(("ps", b),))
                evac(dst[g][:], bank(b), reads=(("ps", b),), writes=((dn, g),))
        for g in range(4):
            b = next_bank()
            mm(bank(b), Wc[:, g, :], T1sb[g][:], True, False, reads=("Wc", ("T1sb", g)), writes=(("ps", b),))
            mm(bank(b), Wsn[:, g, :], T2sb[g][:], False, True, reads=("Wsn", ("T2sb", g)), writes=(("ps", b),))
            evac(ybT[:, g, kb * 512:(kb + 1) * 512], bank(b), reads=(("ps", b),), writes=(("ybT", g),))
    ybT_all = tuple(("ybT", g) for g in range(4))
    if stage == 4:
        dump("ybT", ybT[:], [128, 4, 2048], BF16, ybT_all)
        return finish()

    gbuf = [sb("gbuf%d" % i, [128, 3, 16, 128], BF16, 64 + 16 * i) for i in range(2)]
    ppb = [sb("ppb%d" % i, [128, 8, 128], BF16, 64 + 16 * i + 12) for i in range(2)]
    pfb = [sb("pfb%d" % i, [128, 4, 128], BF16, 64 + 16 * i + 14) for i in range(2)]
    pmb = [sb("pmb%d" % i, [128, 4, 128], BF16, 64 + 16 * i + 15) for i in range(2)]
    sg = [[sb("sg%d_%d" % (i, j), [128, 512], F32, 108.5 + 6 * i + 2 * j) for j in range(3)] for i in range(2)]
    mch = [sb("mch%d" % i, [128, 2048], BF16, 120.5 + 4 * i) for i in range(2)]
    pp_d = inp("proj_pool", [1024, 2048]).rearrange("(c p) n -> p c n", p=128)
    pf_d = inp("proj_fourier", [512, 2048]).rearrange("(c p) n -> p c n", p=128)
    pm_d = inp("proj_mem", [512, 2048]).rearrange("(c p) n -> p c n", p=128)
    mT_d = nc.dram_tensor("mT_scr", [16, 128, 2048], BF16, kind="Internal").ap()
    fdead = zf_all + ("fw_sb", "Wc", "Wsn") + tuple(("T1sb", i) for i in range(4)) + tuple(("T2sb", i) for i in range(4)) + qdead
    P.alias([("sg", i, j) for i in range(2) for j in range(3)] + [("mch", 0), ("mch", 1)], fdead)
    it = 0
    for dc in range(16):
        wb = dc % 2
        wkeys = (("ws", 2 * wb), ("ws", 2 * wb + 1))
        for j in range(3):
            P.dma("pool", "cg%d_%d" % (wb, j), lambda e, wb=wb, j=j, dc=dc: e.dma_start(
                out=gbuf[wb][:, j, :, :], in_=win_d[:, :, 2048 + j * 2048 + dc * 128:2048 + j * 2048 + (dc + 1) * 128]),
                writes=wkeys)
        P.dma("pool", "cpp%d" % wb, lambda e, wb=wb, dc=dc: e.dma_start(out=ppb[wb][:], in_=pp_d[:, :, dc * 128:(dc + 1) * 128]), writes=wkeys)
        P.dma("pool", "cpf%d" % wb, lambda e, wb=wb, dc=dc: e.dma_start(out=pfb[wb][:], in_=pf_d[:, :, dc * 128:(dc + 1) * 128]), writes=wkeys)
        P.dma("pool", "cpm%d" % wb, lambda e, wb=wb, dc=dc: e.dma_start(out=pmb[wb][:], in_=pm_d[:, :, dc * 128:(dc + 1) * 128]), writes=wkeys)
        mi = dc % 2
        for tb in range(4):
            i = it % 2
            it += 1
            tsl = slice(tb * 512, (tb + 1) * 512)
            ybanks = []
            for (wt, src, nk, rk) in ((ppb[wb], yapT, 8, yapT_all), (pfb[wb], ybT, 4, ybT_all), (pmb[wb], ocT, 4, ocT_all)):
                b = next_bank()
                ybanks.append(b)
                for kc in range(nk):
                    mm(bank(b), wt[:, kc, :], src[:, kc, tsl], kc == 0, kc == nk - 1, reads=wkeys + rk, writes=(("ps", b),))
            for j in range(3):
                b = next_bank()
                for c in range(16):
                    mm(bank(b), gbuf[wb][:, j, c, :], hT[:, c, tsl], c == 0, c == 15, reads=wkeys + (("hT", c),), writes=(("ps", b),))
                P.op("act", lambda e, b=b, i=i, j=j: e.activation(out=sg[i][j][:], in_=bank(b), func=AF.Sigmoid),
                     reads=(("ps", b),), writes=(("sg", i, j),))
            for j in range(3):
                P.op("dve", lambda e, i=i, j=j, yb=ybanks[j]: e.tensor_tensor(out=sg[i][j][:], in0=sg[i][j][:], in1=bank(yb), op=ALU.mult),
                     reads=(("sg", i, j), ("ps", ybanks[j])), writes=(("sg", i, j),))
            P.op("dve", lambda e, i=i: e.tensor_tensor(out=sg[i][0][:], in0=sg[i][0][:], in1=sg[i][1][:], op=ALU.add),
                 reads=(("sg", i, 0), ("sg", i, 1)), writes=(("sg", i, 0),))
            P.op("dve", lambda e, i=i, mi=mi, tsl=tsl: e.tensor_tensor(out=mch[mi][:, tsl], in0=sg[i][0][:], in1=sg[i][2][:], op=ALU.add),
                 reads=(("sg", i, 0), ("sg", i, 2)), writes=(("mch", mi),))
        P.dma("sp", "mspill%d" % mi, lambda e, mi=mi, dc=dc: e.dma_start(out=mT_d[dc], in_=mch[mi][:]),
              reads=(("mch", mi),), writes=("mT_d",))
    if stage == 5:
        o = nc.dram_tensor("dbg_mT", [16, 128, 2048], BF16, kind="ExternalOutput").ap()
        stg = sb("dbgstg", [128, 16, 2048], BF16, 0)
        l1 = P.dma("sp", "dbgl", lambda e: e.dma_start(out=stg[:], in_=mT_d.rearrange("c p t -> p c t")), reads=("mT_d",) + hT_all,
                   writes=hT_all)
        final_ops.append(P.dma("sp", "dbgs", lambda e: e.dma_start(out=o.rearrange("c p t -> p c t"), in_=stg[:]), reads=hT_all))
        return finish()

    wout = sb("wout", [128, 16, 2048], BF16, 0)
    mTb = [sb("mTb%d" % i, [128, 16, 512], BF16, 64 + 16 * i) for i in range(2)]
    h2 = sb("h2", [128, 16, 2048], BF16, 104.5)
    xt = [sb("xt%d" % i, [128, 2048], F32, 168.5 + 8 * i) for i in range(2)]
    gffn = sb("gffn", [128, 2048], F32, 184.5)
    h2T = [sb("h2T%d" % i, [128, 16, 128], BF16, 192.5 + 4 * i) for i in range(2)]
    junk = sb("junk", [128, 2048], BF16, 200.5)
    lg = sb("lg", [128, 16], F32, 204.5)
    wout_d = inp("w_out", [2048, 2048]).rearrange("(c p) n -> p c n", p=128)
    x_d = inp("x", [2048, 2048])
    gffn_d = inp("gffn_rep", [128, 2048])
    wr_d = inp("w_router", [2048, 16]).rearrange("(c p) n -> p c n", p=128)
    x1_d = nc.dram_tensor("x1_scr", [2048, 2048], F32, kind="Internal").ap()
    cdead = (tuple(("sg", i, j) for i in range(2) for j in range(3)) + (("mch", 0), ("mch", 1)) + yapT_all + ybT_all + ocT_all
             + ("kT", "vv", "pw_sb"))
    allws = tuple(("ws", s) for s in range(4))
    P.alias([("wout", blk) for blk in range(8)], hT_all)
    P.alias([("xt", 0), ("xt", 1), "gffn", ("h2T", 0), ("h2T", 1), "junk", "lg"] + [("h2", t) for t in range(16)], cdead)
    for blk in range(8):
        P.dma("pool", "wout%d" % (blk % 4), lambda e, blk=blk: e.dma_start(out=wout[:, :, blk * 256:(blk + 1) * 256],
                                                                         in_=wout_d[:, :, blk * 256:(blk + 1) * 256]),
              reads=(), writes=(("wout", blk),))
    wout_all = tuple(("wout", blk) for blk in range(8))
    P.dma("sp", "gffn", lambda e: e.dma_start(out=gffn[:], in_=gffn_d), writes=("gffn",))
    P.dma("pool", "wr", lambda e: e.dma_start(out=wr_bf[:], in_=wr_d), writes=("wr_bf",))
    mT_v = mT_d.rearrange("c p t -> p c t")
    SSQ, INV2, MX, SM = 0, 16, 32, 48
    for tg in range(4):
        mb = tg % 2
        P.dma("sp", "mTb%d" % mb, lambda e, mb=mb, tg=tg: e.dma_start(out=mTb[mb][:], in_=mT_v[:, :, tg * 512:(tg + 1) * 512]),
              reads=("mT_d",), writes=(("ws", 2 * mb), ("ws", 2 * mb + 1)))
        for tt in range(4):
            ti = tg * 4 + tt
            xi = ti % 2
            P.dma("sp", "xt%d" % xi, lambda e, xi=xi, ti=ti: e.dma_start(out=xt[xi][:], in_=x_d[ti * 128:(ti + 1) * 128, :]),
                  writes=(("xt", xi),))
            for db in range(4):
                b = next_bank()
                for c in range(16):
                    mm(bank(b), mTb[mb][:, c, tt * 128:(tt + 1) * 128], wout[:, c, db * 512:(db + 1) * 512], c == 0, c == 15,
                       reads=(("ws", 2 * mb), ("ws", 2 * mb + 1), ("wout", 2 * db), ("wout", 2 * db + 1)), writes=(("ps", b),))
                P.op("dve", lambda e, b=b, xi=xi, db=db: e.tensor_tensor(out=xt[xi][:, db * 512:(db + 1) * 512],
                                                                        in0=xt[xi][:, db * 512:(db + 1) * 512], in1=bank(b), op=ALU.add),
                     reads=(("ps", b), ("xt", xi)), writes=(("xt", xi),))
            P.dma("sp", "x1sp%d" % xi, lambda e, xi=xi, ti=ti: e.dma_start(out=x1_d[ti * 128:(ti + 1) * 128, :], in_=xt[xi][:]),
                  reads=(("xt", xi),), writes=("x1_d",))
            P.op("act", lambda e, xi=xi, ti=ti: e.activation(out=junk[:], in_=xt[xi][:], func=AF.Square,
                                                            accum_out=small[:, SSQ + ti:SSQ + ti + 1]),
                 reads=(("xt", xi),), writes=("junk", ("ssq", ti)))
            P.op("act", lambda e, ti=ti: e.activation(out=small[:, INV2 + ti:INV2 + ti + 1], in_=small[:, SSQ + ti:SSQ + ti + 1],
                                                     func=AF.Sqrt, bias=EPS, scale=1.0 / D),
                 reads=(("ssq", ti),), writes=(("inv2", ti),))
            P.op("dve", lambda e, ti=ti: e.reciprocal(out=small[:, INV2 + ti:INV2 + ti + 1], in_=small[:, INV2 + ti:INV2 + ti + 1]),
                 reads=(("inv2", ti),), writes=(("inv2", ti),))
            P.op("dve", lambda e, xi=xi, ti=ti: e.scalar_tensor_tensor(out=h2[:, ti, :], in0=xt[xi][:], scalar=small[:, INV2 + ti:INV2 + ti + 1],
                                                                      in1=gffn[:], op0=ALU.mult, op1=ALU.mult),
                 reads=(("xt", xi), ("inv2", ti), "gffn"), writes=(("h2", ti),))
            hi_ = ti % 2
            for half in range(2):
                b = next_bank()
                for cc in range(8):
                    c = half * 8 + cc
                    P.op("pe", lambda e, b=b, cc=cc, c=c, ti=ti: e.transpose(bank_bf(b)[:, cc * 128:(cc + 1) * 128],
                                                                             h2[:, ti, c * 128:(c + 1) * 128], ident_bf[:]),
                         reads=(("h2", ti), "ident_bf"), writes=(("ps", b),))
                evac(h2T[hi_][:, half * 8:(half + 1) * 8, :].rearrange("p c t -> p (c t)"), bank_bf(b), reads=(("ps", b),),
                     writes=(("h2T", hi_),))
            b = next_bank()
            for c in range(16):
                mm(bank(b)[:, 0:16], h2T[hi_][:, c, :], wr_bf[:, c, :], c == 0, c == 15, reads=(("h2T", hi_), "wr_bf"), writes=(("ps", b),))
            P.op("dve", lambda e, b=b, ti=ti: e.reduce_max(out=small[:, MX + ti:MX + ti + 1], in_=bank(b)[:, 0:16], axis=AX.X),
                 reads=(("ps", b),), writes=(("mx", ti),))
            P.op("dve", lambda e, ti=ti: e.tensor_scalar(out=small[:, MX + ti:MX + ti + 1], in0=small[:, MX + ti:MX + ti + 1],
                                                        scalar1=-1.0, scalar2=None, op0=ALU.mult),
                 reads=(("mx", ti),), writes=(("mx", ti),))
            P.op("act", lambda e, b=b, ti=ti: e.activation(out=lg[:], in_=bank(b)[:, 0:16], func=AF.Exp,
                                                          bias=small[:, MX + ti:MX + ti + 1], scale=1.0,
                                                          accum_out=small[:, SM + ti:SM + ti + 1]),
                 reads=(("ps", b), ("mx", ti)), writes=("lg", ("sm", ti)))
            P.op("dve", lambda e, ti=ti: e.reciprocal(out=small[:, SM + ti:SM + ti + 1], in_=small[:, SM + ti:SM + ti + 1]),
                 reads=(("sm", ti),), writes=(("sm", ti),))
            P.op("dve", lambda e, ti=ti: e.tensor_scalar(out=aff_tm[:, ti, :], in0=lg[:], scalar1=small[:, SM + ti:SM + ti + 1],
                                                        scalar2=None, op0=ALU.mult),
                 reads=("lg", ("sm", ti)), writes=("aff_tm",))
    h2_all = tuple(("h2", t) for t in range(16))
    if stage == 6:
        dump("h2", h2[:], [128, 16, 2048], BF16, h2_all)
        dump("aff", aff_tm[:], [128, 16, 16], F32, ("aff_tm",))
        return finish()

    affT = sb("affT", [16, 2048], F32, 168.5)
    work = sb("work", [16, 2048], F32, 176.5)
    maskT = sb("maskT", [128, 2048], BF16, 184.5)
    mx8 = sb("mx8", [16, 8], F32, 188.5)
    selT = [sb("selT%d" % i, [128, 16, 512], BF16, 0 + 16 * i) for i in range(2)]
    xin_st = [sb("xin_st%d" % i, [128, 16, 512], BF16, 32 + 16 * i) for i in range(2)]
    xin_d = nc.dram_tensor("xin_scr", [8, 128, 16, 512], BF16, kind="Internal").ap()
    ddead = (("xt", 0), ("xt", 1), "gffn", ("h2T", 0), ("h2T", 1), "junk", "lg")
    P.alias(["affT", "work", "maskT", "mx8"], ddead)
    P.alias([("selT", 0), ("selT", 1), ("xin_st", 0), ("xin_st", 1)], wout_all)
    aff3 = sb("aff3", [128, 3, 16, 32], BF16, 189.0)
    rtmp = sb("rtmp", [128, 16, 16], F32, 192.0)
    P.alias(["aff3", "rtmp"], ddead)
    P.op("pool", lambda e: e.memset(aff3[:], 0.0), writes=("aff3",))
    P.op("dve", lambda e: e.tensor_copy(out=aff3[:, 0, :, 0:16], in_=aff_tm[:]), reads=("aff_tm",), writes=("aff3",))
    P.op("dve", lambda e: e.tensor_tensor(out=rtmp[:], in0=aff_tm[:], in1=aff3[:, 0, :, 0:16], op=ALU.subtract), reads=("aff_tm", "aff3"), writes=("rtmp",))
    P.op("dve", lambda e: e.tensor_copy(out=aff3[:, 1, :, 0:16], in_=rtmp[:]), reads=("rtmp",), writes=("aff3",))
    P.op("dve", lambda e: e.tensor_tensor(out=rtmp[:], in0=rtmp[:], in1=aff3[:, 1, :, 0:16], op=ALU.subtract), reads=("rtmp", "aff3"), writes=("rtmp",))
    P.op("dve", lambda e: e.tensor_copy(out=aff3[:, 2, :, 0:16], in_=rtmp[:]), reads=("rtmp",), writes=("aff3",))
    if stage == 7.05:
        dump("aff3", aff3[:], [128, 3, 16, 32], BF16, ("aff3",))
        return finish()
    bks = [next_bank() for _ in range(4)]
    for ti in range(16):
        b = bks[ti // 4]
        off = (ti % 4) * 128
        for k3 in range(3):
            mm(bank(b)[0:32, off:off + 128], aff3[:, k3, ti, :], ident_bf[:], k3 == 0, k3 == 2,
               reads=("aff3", "ident_bf"), writes=(("ps", b),))
    for q in range(4):
        P.op("act", lambda e, q=q, bks=bks: e.copy(out=affT[:, q * 512:(q + 1) * 512], in_=bank(bks[q])[0:16, :]),
             reads=(("ps", bks[q]),), writes=("affT",))
    P.op("dve", lambda e: e.tensor_copy(out=work[:], in_=affT[:]), reads=("affT",), writes=("work",))
    if stage == 7.1:
        dump("affT", affT[:], [16, 2048], F32, ("affT",))
        return finish()
    LO, TT, CNT, FLG = 0, 1, 2, 3
    P.op("dve", lambda e: e.memset(mx8[:], 0.0), writes=("mx8",))
    for kbit in range(26):
        step = float(2.0 ** -(kbit + 1))
        P.op("dve", lambda e, step=step: e.tensor_scalar(out=work[:], in0=affT[:], scalar1=mx8[:, LO:LO + 1], scalar2=step,
                                                        op0=ALU.subtract, op1=ALU.is_ge),
             reads=("mx8", "affT"), writes=("work",))
        P.op("dve", lambda e: e.reduce_sum(out=mx8[:, CNT:CNT + 1], in_=work[:], axis=AX.X), reads=("work",), writes=("mx8",))
        P.op("dve", lambda e, step=step: e.tensor_scalar(out=mx8[:, FLG:FLG + 1], in0=mx8[:, CNT:CNT + 1], scalar1=float(CAP) - 0.5,
                                                        scalar2=step, op0=ALU.is_ge, op1=ALU.mult),
             reads=("mx8",), writes=("mx8",))
        P.op("dve", lambda e: e.tensor_tensor(out=mx8[:, LO:LO + 1], in0=mx8[:, LO:LO + 1], in1=mx8[:, FLG:FLG + 1], op=ALU.add),
             reads=("mx8",), writes=("mx8",))
    if stage == 7.2:
        dump("mx8", mx8[:], [16, 8], F32, ("mx8",))
        return finish()
    P.op("pool", lambda e: e.memset(maskT[:], 0.0), writes=("maskT",))
    P.op("dve", lambda e: e.tensor_scalar(out=maskT[0:16, :], in0=affT[:], scalar1=mx8[:, 0:1], scalar2=None, op0=ALU.is_ge),
         reads=("affT", "mx8"), writes=("maskT",))
    if stage == 7.3:
        dump("maskT", maskT[0:16, :], [16, 2048], BF16, ("maskT",))
        return finish()
    b = next_bank()
    for ti in range(16):
        mm(bank(b)[:, ti * 16:(ti + 1) * 16], maskT[:, ti * 128:(ti + 1) * 128], ident_bf[:, 0:16], True, True,
           reads=("maskT", "ident_bf"), writes=(("ps", b),))
    P.op("act", lambda e, b=b: e.copy(out=mask_tm[:].rearrange("p a b -> p (a b)"), in_=bank(b)[:, 0:256]),
         reads=(("ps", b),), writes=("mask_tm",))
    if stage == 7.35:
        dump("mask", mask_tm[:], [128, 16, 16], F32, ("mask_tm",))
        return finish()
    P.op("dve", lambda e: e.tensor_copy(out=maskbf_tm[:], in_=mask_tm[:]), reads=("mask_tm",), writes=("maskbf_tm",))
    if stage == 7.4:
        dump("mask", mask_tm[:], [128, 16, 16], F32, ("mask_tm",))
        return finish()
    P.op("dve", lambda e: e.tensor_tensor(out=A_tm[:].rearrange("p a b -> p (a b)"), in0=mask_tm[:].rearrange("p a b -> p (a b)"),
                                          in1=aff_tm[:].rearrange("p a b -> p (a b)"), op=ALU.mult),
         reads=("mask_tm", "aff_tm"), writes=("A_tm",))
    b = next_bank()
    for ti in range(16):
        for j in range(ti + 1):
            lhs = tri_bf if j == ti else ones_bf
            mm(bank(b)[:, ti * 16:(ti + 1) * 16], lhs[:], maskbf_tm[:, j, :], j == 0, j == ti,
               reads=("maskbf_tm", "tri_bf", "ones_bf"), writes=(("ps", b),))
    P.op("act", lambda e, b=b: e.copy(out=pos_tm[:].rearrange("p a b -> p (a b)"), in_=bank(b)[:, 0:256]),
         reads=(("ps", b),), writes=("pos_tm",))
    if stage == 7:
        dump("mask", mask_tm[:], [128, 16, 16], F32, ("mask_tm",))
        dump("pos", pos_tm[:], [128, 16, 16], F32, ("pos_tm",))
        dump("A", A_tm[:], [128, 16, 16], F32, ("A_tm",))
        return finish()

    def onehot(ep):
        si = ep % 2
        for ti in range(16):
            for ee in range(2):
                e_ = 2 * ep + ee
                P.op("dve", lambda e, si=si, ti=ti, ee=ee, e_=e_: e.tensor_scalar(
                    out=selT[si][:, ti, ee * 256:(ee + 1) * 256], in0=iota_f[:], scalar1=pos_tm[:, ti, e_:e_ + 1],
                    scalar2=mask_tm[:, ti, e_:e_ + 1], op0=ALU.is_equal, op1=ALU.mult),
                    reads=("iota_f", "pos_tm", "mask_tm"), writes=(("selT", si),))

    onehot(0)
    for ep in range(8):
        si = ep % 2
        if ep + 1 < 8:
            onehot(ep + 1)
        for c in range(16):
            b = next_bank()
            for ti in range(16):
                mm(bank(b), h2[:, ti, c * 128:(c + 1) * 128], selT[si][:, ti, :], ti == 0, ti == 15,
                   reads=(("selT", si), ("h2", ti)), writes=(("ps", b),))
            P.op("act", lambda e, si=si, c=c, b=b: e.copy(out=xin_st[si][:, c, :], in_=bank(b)), reads=(("ps", b),),
                 writes=(("xin_st", si),))
        P.dma("sp", "xinsp%d" % si, lambda e, si=si, ep=ep: e.dma_start(out=xin_d[ep], in_=xin_st[si][:]),
              reads=(("xin_st", si),), writes=(("xin_d", ep),))
    if stage == 8:
        o = nc.dram_tensor("dbg_xin", [8, 128, 16, 512], BF16, kind="ExternalOutput").ap()
        for ep in range(8):
            si = ep % 2
            P.dma("sp", "dbgl%d" % si, lambda e, si=si, ep=ep: e.dma_start(out=xin_st[si][:], in_=xin_d[ep]),
                  reads=(("xin_d", ep),), writes=(("xin_st", si),))
            final_ops.append(P.dma("sp", "dbgs%d" % si, lambda e, si=si, ep=ep: e.dma_start(out=o[ep], in_=xin_st[si][:]),
                                   reads=(("xin_st", si),)))
        return finish()

    accA = sb("accA", [128, 8, 2048], F32, 0)
    accB = sb("accB", [128, 8, 2048], F32, 104.5)

    def acc(ti):
        return (accA if ti < 8 else accB)[:, ti % 8, :]

    xinT = sb("xinT", [128, 16, 256], BF16, 168.5)
    hidT = [sb("hidT%d" % i, [128, 16, 256], BF16, 176.5 + 8 * i) for i in range(2)]
    selA = sb("selA", [128, 2, 2048], BF16, 192.5)
    selAT = [sb("selAT%d" % i, [128, 4, 256], BF16, 200.5 + 2 * i) for i in range(2)]
    ysb = sb("ysb", [128, 2, 512], BF16, 204.5)
    sil = sb("sil", [128, 256], F32, 206.5)
    wd8 = [sb("wd8_%d" % i, [128, 8, 512], BF16, 64 + 8 * i) for i in range(4)]
    wg_d = inp("w_gate", [16, 2048, 2048])
    wu_d = inp("w_up", [16, 2048, 2048])
    wd_d = inp("w_down", [16, 2048, 2048])
    e0dead = (("selT", 0), ("selT", 1), ("xin_st", 0), ("xin_st", 1)) + h2_all + ("affT", "work", "maskT", "mx8")
    P.alias([("acc", t) for t in range(16)] + ["xinT", ("hidT", 0), ("hidT", 1), "selA", ("selAT", 0), ("selAT", 1),
             ("ysb", 0), ("ysb", 1), "sil"], e0dead + ddead)
    P.dma("sp", "xinld", lambda e: e.dma_start(out=xinT[:], in_=xin_d[0][:, :, 0:256]), reads=(("xin_d", 0),), writes=("xinT",))
    for ti in range(16):
        P.dma("sp", "accld%d" % (ti % 4), lambda e, ti=ti: e.dma_start(out=acc(ti), in_=x1_d[ti * 128:(ti + 1) * 128, :]),
              reads=("x1_d",), writes=(("acc", ti),))
    wviews = {}

    def wv(ex):
        if ex not in wviews:
            wviews[ex] = (wg_d[ex].rearrange("(c p) n -> p c n", p=128), wu_d[ex].rearrange("(c p) n -> p c n", p=128),
                          wd_d[ex].rearrange("(c p) n -> p c n", p=128))
        return wviews[ex]

    def xin_load(ex):
        ep, ee = ex // 2, ex % 2
        P.dma("sp", "xinld", lambda e, ep=ep, ee=ee: e.dma_start(out=xinT[:], in_=xin_d[ep][:, :, ee * 256:(ee + 1) * 256]),
              reads=(("xin_d", ep),), writes=("xinT",))

    def gate_up_step(ex, fp):
        wgv, wuv, _ = wv(ex)
        hi_ = ex % 2
        sg_ = next_slot()
        P.dma("pool", "ws%d" % sg_, lambda e, s=sg_, fp=fp, wgv=wgv: e.dma_start(out=wslot[s][:], in_=wgv[:, :, fp * 256:(fp + 1) * 256]),
              writes=(("ws", sg_),))
        su_ = next_slot()
        P.dma("pool", "ws%d" % su_, lambda e, s=su_, fp=fp, wuv=wuv: e.dma_start(out=wslot[s][:], in_=wuv[:, :, fp * 256:(fp + 1) * 256]),
              writes=(("ws", su_),))
        for f2 in range(2):
            fc = fp * 2 + f2
            bg = next_bank()
            for c in range(16):
                mm(bank(bg)[:, 0:256], wslot[sg_][:, c, f2 * 128:(f2 + 1) * 128], xinT[:, c, :], c == 0, c == 15,
                   reads=(("ws", sg_), "xinT"), writes=(("ps", bg),))
            bu = next_bank()
            for c in range(16):
                mm(bank(bu)[:, 0:256], wslot[su_][:, c, f2 * 128:(f2 + 1) * 128], xinT[:, c, :], c == 0, c == 15,
                   reads=(("ws", su_), "xinT"), writes=(("ps", bu),))
            P.op("act", lambda e, bg=bg: e.activation(out=sil[:], in_=bank(bg)[:, 0:256], func=AF.Silu),
                 reads=(("ps", bg),), writes=("sil",))
            P.op("dve", lambda e, bu=bu, hi_=hi_, fc=fc: e.tensor_tensor(out=hidT[hi_][:, fc, :], in0=sil[:], in1=bank(bu)[:, 0:256],
                                                                        op=ALU.mult),
                 reads=("sil", ("ps", bu)), writes=(("hidT", hi_),))

    def sel_build(ex):
        for tq in range(4):
            sti = tq % 2
            for t4 in range(4):
                ti = tq * 4 + t4
                P.op("dve", lambda e, sti=sti, t4=t4, ti=ti, ex=ex: e.tensor_scalar(
                    out=selAT[sti][:, t4, :], in0=iota_f[:], scalar1=pos_tm[:, ti, ex:ex + 1], scalar2=A_tm[:, ti, ex:ex + 1],
                    op0=ALU.is_equal, op1=ALU.mult),
                    reads=("iota_f", "pos_tm", "A_tm"), writes=(("selAT", sti),))
            for sc in range(2):
                b = next_bank()
                for t4 in range(4):
                    P.op("pe", lambda e, b=b, t4=t4, sti=sti, sc=sc: e.transpose(bank_bf(b)[:, t4 * 128:(t4 + 1) * 128],
                                                                               selAT[sti][:, t4, sc * 128:(sc + 1) * 128], ident_bf[:]),
                         reads=(("selAT", sti), "ident_bf"), writes=(("ps", b),))
                evac(selA[:, sc, tq * 512:(tq + 1) * 512], bank_bf(b)[:, 0:512], reads=(("ps", b),), writes=("selA",))

    def down_step(ex, db, after_tile=None):
        _, _, wdv = wv(ex)
        hi_ = ex % 2
        sd = []
        for hf in range(2):
            s = next_slot()
            sd.append(s)
            P.dma("pool", "ws%d" % s, lambda e, s=s, hf=hf, db=db, wdv=wdv: e.dma_start(
                out=wd8[s][:], in_=wdv[:, hf * 8:(hf + 1) * 8, db * 512:(db + 1) * 512]), writes=(("ws", s),))
        for sc in range(2):
            b = next_bank()
            for fc in range(16):
                s = sd[fc // 8]
                mm(bank(b), hidT[hi_][:, fc, sc * 128:(sc + 1) * 128], wd8[s][:, fc % 8, :], fc == 0, fc == 15,
                   reads=(("ws", s), ("hidT", hi_)), writes=(("ps", b),))
            evac(ysb[:, sc, :], bank(b), reads=(("ps", b),), writes=(("ysb", sc),))
        for ti in range(16):
            b = next_bank()
            for sc in range(2):
                mm(bank(b), selA[:, sc, ti * 128:(ti + 1) * 128], ysb[:, sc, :], sc == 0, sc == 1,
                   reads=("selA", ("ysb", 0), ("ysb", 1)), writes=(("ps", b),))
            P.op("dve", lambda e, b=b, ti=ti, db=db: e.tensor_tensor(out=acc(ti)[:, db * 512:(db + 1) * 512],
                                                                    in0=acc(ti)[:, db * 512:(db + 1) * 512], in1=bank(b), op=ALU.add),
                 reads=(("ps", b), ("acc", ti)), writes=(("acc", ti),))
            if after_tile is not None:
                after_tile(ti)

    gfin = sb("gfin", [128, 2048], F32, 168.5)
    junk2 = sb("junk2", [128, 2048], BF16, 176.5)
    gfin_d = inp("gfin_rep", [128, 2048])
    out_d = nc.dram_tensor("out", [2048, 2048], F32, kind="ExternalOutput").ap()
    FSS, FINV = 0, 16

    def final_tile(ti):
        P.op("act", lambda e, ti=ti: e.activation(out=junk2[:], in_=acc(ti), func=AF.Square, accum_out=small[:, FSS + ti:FSS + ti + 1]),
             reads=(("acc", ti),), writes=("junk2", ("fss", ti), ("ssq", ti)))
        P.op("act", lambda e, ti=ti: e.activation(out=small[:, FINV + ti:FINV + ti + 1], in_=small[:, FSS + ti:FSS + ti + 1],
                                                 func=AF.Sqrt, bias=EPS, scale=1.0 / D),
             reads=(("fss", ti),), writes=(("finv", ti), ("inv2", ti)))
        P.op("dve", lambda e, ti=ti: e.reciprocal(out=small[:, FINV + ti:FINV + ti + 1], in_=small[:, FINV + ti:FINV + ti + 1]),
             reads=(("finv", ti),), writes=(("finv", ti),))
        P.op("dve", lambda e, ti=ti: e.scalar_tensor_tensor(out=acc(ti), in0=acc(ti), scalar=small[:, FINV + ti:FINV + ti + 1],
                                                           in1=gfin[:], op0=ALU.mult, op1=ALU.mult),
             reads=(("acc", ti), ("finv", ti), "gfin"), writes=(("acc", ti),))
        final_ops.append(P.dma("sp", "ost%d" % (ti % 4), lambda e, ti=ti: e.dma_start(out=out_d[ti * 128:(ti + 1) * 128, :], in_=acc(ti)),
                               reads=(("acc", ti),)))

    for ex in range(E + 1):
        if 0 < ex < E:
            xin_load(ex)
        if ex == E:
            P.alias(["gfin"], ["xinT"])
            P.alias(["junk2"], [("hidT", 0)])
            P.dma("sp", "gfin", lambda e: e.dma_start(out=gfin[:], in_=gfin_d), writes=("gfin",))
        for step in range(8):
            if ex < E:
                gate_up_step(ex, step)
            if ex >= 1:
                if step == 0:
                    sel_build(ex - 1)
                if step % 2 == 1:
                    down_step(ex - 1, step // 2, after_tile=final_tile if (ex == E and step == 7) else None)
    return finish()


def _consts():
    mats = np.zeros((6, 128, 128), np.float32)
    mats[0] = 1.0
    mats[1] = np.eye(128, dtype=np.float32)
    mats[2] = np.triu(np.ones((128, 128), np.float32), k=1)
    k = np.arange(128, dtype=np.float64)
    ang = 2 * np.pi * np.outer(k, k) / 128.0
    mats[3] = (np.cos(ang) / np.sqrt(128.0)).astype(np.float32)
    mats[4] = (-np.sin(ang) / np.sqrt(128.0)).astype(np.float32)
    iota = np.tile(np.arange(256, dtype=np.float32)[None, :], (128, 1))
    s = np.arange(S, dtype=np.int64)
    angS = 2 * np.pi * ((s[:, None] * s[None, :]) % S).astype(np.float64) / S
    cosS = (np.cos(angS) / np.sqrt(float(S))).astype(np.float32)
    sinS = (np.sin(angS) / np.sqrt(float(S))).astype(np.float32)
    invc = np.zeros((4, 128, S), np.float32)
    for g, w in enumerate((2, 4, 8, 16)):
        lo = np.clip(s - w // 2, 0, S)
        hi = np.clip(s + w - w // 2, 0, S)
        invc[g] = (1.0 / (hi - lo).astype(np.float64)).astype(np.float32)[None, :]
    return mats, iota, cosS, sinS, invc


def make_in_maps(inputs, in_shapes, cores):
    mats, iota, cosS, sinS, invc = _consts()
    f = lambda a: np.ascontiguousarray(np.asarray(a, dtype=np.float32))
    vec = np.zeros((128, 40), np.float32)
    vec[:, 0:16] = f(inputs["norm_mix_g"])[0].reshape(16, 128).T
    vec[:, 16:32] = f(inputs["norm_mem_g"])[0].reshape(16, 128).T
    vec[:, 32:40] = f(inputs["pool_scale"])[0].reshape(8, 128).T
    shared = {
        "cst_mats": mats, "cst_iota": iota, "cst_vecs": vec, "cst_cosS": cosS, "cst_sinS": sinS, "cst_invc": invc,
        "w_kv": f(inputs["w_kv_mem"])[0], "w_in": f(inputs["w_in"])[0],
        "pool_w": f(inputs["pool_w"])[0].reshape(1024, 256), "fourier_w": f(inputs["fourier_w"])[0].reshape(512, 128),
        "proj_pool": f(inputs["proj_pool"])[0], "proj_fourier": f(inputs["proj_fourier"])[0], "proj_mem": f(inputs["proj_mem"])[0],
        "w_out": f(inputs["w_out"])[0], "w_router": f(inputs["w_router"])[0],
        "gffn_rep": np.ascontiguousarray(np.tile(f(inputs["norm_ffn_g"])[0][None, :], (128, 1))),
        "gfin_rep": np.ascontiguousarray(np.tile(f(inputs["norm_final_g"])[None, :], (128, 1))),
        "w_gate": f(inputs["w_expert_gate"])[0], "w_up": f(inputs["w_expert_up"])[0], "w_down": f(inputs["w_expert_down"])[0],
    }
    x = f(inputs["x"])
    mem = f(inputs["mem"])
    maps = []
    for b in cores:
        m = {}
        for name in in_shapes:
            if name == "x":
                m[name] = x[b]
            elif name == "xT":
                m[name] = np.ascontiguousarray(x[b].T)
            elif name == "memT":
                m[name] = np.ascontiguousarray(mem[b].T)
            else:
                m[name] = shared[name]
            assert tuple(m[name].shape) == tuple(in_shapes[name]), (name, m[name].shape, in_shapes[name])
        maps.append(m)
    return maps


def kernel(**inputs):
    nc, in_shapes = build()
    maps = make_in_maps(inputs, in_shapes, list(range(8)))
    res = run_bass_kernel_spmd(nc, maps, core_ids=list(range(8)))
    return np.stack([np.asarray(r["out"], dtype=np.float32) for r in res.results], axis=0)
```

```python
import numpy as np
import concourse.bass as bass
import concourse.mybir as mybir
from concourse.bass_utils import run_bass_kernel_spmd

F32 = mybir.dt.float32
BF16 = mybir.dt.bfloat16
AF = mybir.ActivationFunctionType
ALU = mybir.AluOpType
AX = mybir.AxisListType

SAME_ENGINE_SYNC = True
S = 2048
D = 2048
E = 16
CAP = 256
EPS = 1e-6
BASE = 16640


class Op:
    __slots__ = ("eng", "fn", "is_dma", "chan", "needs_inc", "deps", "cnt")

    def __init__(self, eng, fn, is_dma=False, chan=None):
        self.eng = eng
        self.fn = fn
        self.is_dma = is_dma
        self.chan = chan
        self.needs_inc = is_dma
        self.deps = []
        self.cnt = None


class Plan:
    ENGS = ("pe", "act", "dve", "pool", "sp")

    def __init__(self, nc):
        self.nc = nc
        self.ops = {e: [] for e in self.ENGS}
        self.last_w = {}
        self.readers = {}
        self.chan_last = {}
        self.chan_count = {}

    def _add(self, op, reads, writes):
        deps = {}
        for r in reads:
            w = self.last_w.get(r)
            if w is not None:
                deps[id(w)] = w
        for k in writes:
            w = self.last_w.get(k)
            if w is not None:
                deps[id(w)] = w
            for rd in self.readers.get(k, ()):
                deps[id(rd)] = rd
        for r in reads:
            self.readers.setdefault(r, []).append(op)
        for k in writes:
            self.last_w[k] = op
            self.readers[k] = []
        deps.pop(id(op), None)
        for d in deps.values():
            if d.is_dma:
                op.deps.append(d)
            elif d.eng == op.eng and not op.is_dma:
                if d.eng != "pe" and SAME_ENGINE_SYNC:
                    d.needs_inc = True
                    op.deps.append(d)
            else:
                d.needs_inc = True
                op.deps.append(d)
        self.ops[op.eng].append(op)
        return op

    def alias(self, new_keys, dead_keys):
        ops = {}
        for k in dead_keys:
            w = self.last_w.get(k)
            if w is not None:
                ops[id(w)] = w
            for r in self.readers.get(k, ()):
                ops[id(r)] = r
        for nk in new_keys:
            lst = self.readers.setdefault(nk, [])
            have = {id(o) for o in lst}
            for o in ops.values():
                if id(o) not in have:
                    lst.append(o)

    def op(self, eng, fn, reads=(), writes=()):
        return self._add(Op(eng, fn), reads, writes)

    def dma(self, queue, chan, fn, reads=(), writes=()):
        op = Op(queue, fn, is_dma=True, chan=chan)
        prev = self.chan_last.get(chan)
        if prev is not None:
            op.deps.append(prev)
        self.chan_last[chan] = op
        c = self.chan_count.get(chan, 0) + 1
        self.chan_count[chan] = c
        op.cnt = 16 * c
        return self._add(op, reads, writes)

    def emit(self, final_waits=()):
        nc = self.nc
        for e in self.ENGS:
            c = 0
            for op in self.ops[e]:
                if op.is_dma:
                    continue
                if op.needs_inc:
                    c += 1
                    op.cnt = c
        sems = {}

        def sem_of(key):
            if key not in sems:
                sems[key] = nc.alloc_semaphore("s_%d" % len(sems))
            return sems[key]

        def key_of(d):
            return ("ch", d.chan) if d.is_dma else ("eng", d.eng)

        plan = self

        def run(engname, eng):
            waited = {}
            for op in plan.ops[engname]:
                need = {}
                for d in op.deps:
                    k = key_of(d)
                    if d.cnt > need.get(k, 0):
                        need[k] = d.cnt
                for k, v in need.items():
                    if waited.get(k, 0) >= v:
                        continue
                    eng.wait_ge(sem_of(k), v)
                    waited[k] = v
                ins = op.fn(eng)
                if op.is_dma:
                    ins.then_inc(sem_of(("ch", op.chan)), 16)
                elif op.needs_inc:
                    ins.then_inc(sem_of(("eng", engname)), 1)
            if engname == "sp":
                for d in final_waits:
                    eng.wait_ge(sem_of(key_of(d)), d.cnt)

        with nc.Block() as block:
            @block.tensor
            def _(eng):
                run("pe", eng)

            @block.scalar
            def _(eng):
                run("act", eng)

            @block.vector
            def _(eng):
                run("dve", eng)

            @block.gpsimd
            def _(eng):
                run("pool", eng)

            @block.sync
            def _(eng):
                run("sp", eng)


def build(stage=99, dbg=None):
    nc = bass.Bass("TRN2", target_bir_lowering=False)
    P = Plan(nc)
    in_shapes = {}

    def inp(name, shape):
        in_shapes[name] = tuple(shape)
        return nc.dram_tensor(name, list(shape), F32, kind="ExternalInput").ap()

    def sb(name, shape, dt, kib):
        return nc.alloc_sbuf_tensor_at(name, list(shape), dt, offset=BASE + int(round(kib * 1024)))

    psA = nc.alloc_psum_tensor("psA", [128, 2048], F32)
    psB = nc.alloc_psum_tensor("psB", [128, 2048], F32)

    def bank(i):
        t = psA if i < 4 else psB
        j = i % 4
        return t[:, j * 512:(j + 1) * 512]

    psA_bf = psA.bitcast(BF16)
    psB_bf = psB.bitcast(BF16)

    def bank_bf(i):
        t = psA_bf if i < 4 else psB_bf
        j = i % 4
        return t[:, j * 1024:(j + 1) * 1024]

    bank_ctr = [0]

    def next_bank():
        b = bank_ctr[0] % 8
        bank_ctr[0] += 1
        return b

    cp_ctr = [0]

    def evac(out_ap, in_ap, reads, writes, scale=None):
        cp_ctr[0] += 1
        if scale is not None:
            return P.op("act", lambda e: e.activation(out=out_ap, in_=in_ap, func=AF.Identity, scale=scale),
                        reads=reads, writes=writes)
        if cp_ctr[0] % 2 == 0:
            return P.op("act", lambda e: e.copy(out=out_ap, in_=in_ap), reads=reads, writes=writes)
        return P.op("dve", lambda e: e.tensor_copy(out=out_ap, in_=in_ap), reads=reads, writes=writes)

    def mm(out_ap, lhsT, rhs, start, stop, reads, writes):
        return P.op("pe", lambda e: e.matmul(out_ap, lhsT, rhs, start=start, stop=stop), reads=reads, writes=writes)

    wslot = [sb("wslot%d" % i, [128, 16, 256], BF16, 64 + 8 * i) for i in range(4)]
    slot_ctr = [0]

    def next_slot():
        s = slot_ctr[0] % 4
        slot_ctr[0] += 1
        return s

    CK = 96.0
    ones_bf = sb("ones_bf", [128, 128], BF16, CK)
    ident_bf = sb("ident_bf", [128, 128], BF16, CK + 0.25)
    tri_bf = sb("tri_bf", [128, 128], BF16, CK + 0.5)
    c128_bf = sb("c128_bf", [128, 128], BF16, CK + 0.75)
    ns128_bf = sb("ns128_bf", [128, 128], BF16, CK + 1.0)
    ident_f = sb("ident_f", [128, 128], F32, CK + 1.25)
    iota_f = sb("iota_f", [128, 256], F32, CK + 1.75)
    gmix = sb("gmix", [128, 16], F32, CK + 2.75)
    gmem = sb("gmem", [128, 16], F32, CK + 2.8125)
    pscale = sb("pscale", [128, 8], F32, CK + 2.875)
    wr_bf = sb("wr_bf", [128, 16, 16], BF16, CK + 3.0)
    aff_tm = sb("aff_tm", [128, 16, 16], F32, CK + 3.5)
    mask_tm = sb("mask_tm", [128, 16, 16], F32, CK + 4.5)
    pos_tm = sb("pos_tm", [128, 16, 16], F32, CK + 5.5)
    A_tm = sb("A_tm", [128, 16, 16], F32, CK + 6.5)
    small = sb("small", [128, 64], F32, CK + 7.5)
    maskbf_tm = sb("maskbf_tm", [128, 16, 16], BF16, CK + 7.75)

    consts = inp("cst_mats", [6, 128, 128])
    iota_d = inp("cst_iota", [128, 256])
    vec_d = inp("cst_vecs", [128, 40])

    for i, t in enumerate((ones_bf, ident_bf, tri_bf, c128_bf, ns128_bf)):
        P.dma("pool", "cst%d" % i, lambda e, t=t, i=i: e.dma_start(out=t[:], in_=consts[i]), writes=(t.name,))
    P.dma("sp", "cstf", lambda e: e.dma_start(out=ident_f[:], in_=consts[1]), writes=("ident_f",))
    P.dma("sp", "csti", lambda e: e.dma_start(out=iota_f[:], in_=iota_d), writes=("iota_f",))
    P.dma("sp", "cstv0", lambda e: e.dma_start(out=gmix[:], in_=vec_d[:, 0:16]), writes=("gmix",))
    P.dma("sp", "cstv1", lambda e: e.dma_start(out=gmem[:], in_=vec_d[:, 16:32]), writes=("gmem",))
    P.dma("sp", "cstv2", lambda e: e.dma_start(out=pscale[:], in_=vec_d[:, 32:40]), writes=("pscale",))

    dbg_out = {}
    final_ops = []

    def dump(name, tensor_ap, shape, dt, reads):
        o = nc.dram_tensor("dbg_" + name, list(shape), dt, kind="ExternalOutput").ap()
        dbg_out[name] = True
        final_ops.append(P.dma("sp", "dbg_" + name, lambda e: e.dma_start(out=o, in_=tensor_ap), reads=reads))

    def finish():
        P.emit(final_waits=final_ops)
        return nc, in_shapes

    kT = sb("kT", [128, 4, 256], BF16, 104.5)
    vv = sb("vv", [128, 2, 512], BF16, 106.5)
    T0 = 172.5
    memst = sb("memst", [128, 16, 256], F32, T0)
    memsq = sb("memsq", [128, 16, 256], BF16, T0 + 16)
    invBm = sb("invBm", [128, 256], F32, T0 + 24)
    memnT = sb("memnT", [128, 16, 256], BF16, T0 + 25)

    memT_d = inp("memT", [2048, 256]).rearrange("(c p) m -> p c m", p=128)
    wkv_d = inp("w_kv", [2048, 1024]).rearrange("(c p) n -> p c n", p=128)

    P.dma("sp", "memst", lambda e: e.dma_start(out=memst[:], in_=memT_d), writes=("memst",))
    P.op("act", lambda e: e.activation(out=memsq[:], in_=memst[:], func=AF.Square), reads=("memst",), writes=("memsq",))
    b = next_bank()
    for c in range(16):
        mm(bank(b)[:, 0:256], ones_bf[:], memsq[:, c, :], c == 0, c == 15,
           reads=("memsq", "ones_bf"), writes=(("ps", b),))
    P.op("act", lambda e, b=b: e.activation(out=invBm[:], in_=bank(b)[:, 0:256], func=AF.Sqrt, bias=EPS, scale=1.0 / D),
         reads=(("ps", b),), writes=("invBm",))
    P.op("dve", lambda e: e.reciprocal(out=invBm[:], in_=invBm[:]), reads=("invBm",), writes=("invBm",))
    for c in range(16):
        P.op("dve", lambda e, c=c: e.scalar_tensor_tensor(out=memnT[:, c, :], in0=memst[:, c, :], scalar=gmem[:, c:c + 1],
                                                         in1=invBm[:], op0=ALU.mult, op1=ALU.mult),
             reads=("memst", "invBm", "gmem"), writes=(("memnT", c),))
    memnT_all = tuple(("memnT", c) for c in range(16))
    for blk in range(4):
        s = next_slot()
        P.dma("pool", "ws%d" % s, lambda e, s=s, blk=blk: e.dma_start(out=wslot[s][:], in_=wkv_d[:, :, blk * 256:(blk + 1) * 256]),
              writes=(("ws", s),))
        if blk < 2:
            for hh in range(2):
                h = 2 * blk + hh
                b = next_bank()
                for c in range(16):
                    mm(bank(b)[:, 0:256], wslot[s][:, c, hh * 128:(hh + 1) * 128], memnT[:, c, :], c == 0, c == 15,
                       reads=(("ws", s),) + memnT_all, writes=(("ps", b),))
                evac(kT[:, h, :], bank(b)[:, 0:256], reads=(("ps", b),), writes=("kT",))
        else:
            for mc in range(2):
                b = next_bank()
                for c in range(16):
                    mm(bank(b)[:, 0:256], memnT[:, c, mc * 128:(mc + 1) * 128], wslot[s][:, c, :], c == 0, c == 15,
                       reads=(("ws", s),) + memnT_all, writes=(("ps", b),))
                evac(vv[:, mc, (blk - 2) * 256:(blk - 1) * 256], bank(b)[:, 0:256], reads=(("ps", b),), writes=("vv",))
    if stage == 0:
        dump("kT", kT[:], [128, 4, 256], BF16, ("kT",))
        dump("vv", vv[:], [128, 2, 512], BF16, ("vv",))
        return finish()

    hT = sb("hT", [128, 16, 2048], BF16, 0)
    xst = [sb("xst%d" % i, [128, 2048], F32, T0 + 8 * i) for i in range(2)]
    sq = [sb("sq%d" % i, [128, 2048], BF16, T0 + 16 + 4 * i) for i in range(2)]
    invB = sb("invB", [128, 2048], F32, T0 + 24)
    xT_d = inp("xT", [2048, 2048]).rearrange("(c p) t -> p c t", p=128)

    P.alias([("xst", 0), ("xst", 1), ("sq", 0), ("sq", 1), "invB"], memnT_all + ("memst", "memsq", "invBm"))
    bks = [next_bank() for _ in range(4)]
    for c in range(16):
        i = c % 2
        P.dma("sp", "xst%d" % i, lambda e, i=i, c=c: e.dma_start(out=xst[i][:], in_=xT_d[:, c, :]),
              writes=(("xst", i),))
        P.op("act", lambda e, i=i: e.activation(out=sq[i][:], in_=xst[i][:], func=AF.Square),
             reads=(("xst", i),), writes=(("sq", i),))
        for tb in range(4):
            mm(bank(bks[tb]), ones_bf[:], sq[i][:, tb * 512:(tb + 1) * 512], c == 0, c == 15,
               reads=(("sq", i), "ones_bf"), writes=(("ps", bks[tb]),))
    for tb in range(4):
        P.op("act", lambda e, tb=tb, bks=bks: e.activation(out=invB[:, tb * 512:(tb + 1) * 512], in_=bank(bks[tb]), func=AF.Sqrt,
                                                 bias=EPS, scale=1.0 / D),
             reads=(("ps", bks[tb]),), writes=("invB",))
    P.op("dve", lambda e: e.reciprocal(out=invB[:], in_=invB[:]), reads=("invB",), writes=("invB",))
    for c in range(16):
        i = c % 2
        P.dma("sp", "xst%d" % i, lambda e, i=i, c=c: e.dma_start(out=xst[i][:], in_=xT_d[:, c, :]), writes=(("xst", i),))
        P.op("dve", lambda e, i=i, c=c: e.scalar_tensor_tensor(out=hT[:, c, :], in0=xst[i][:], scalar=gmix[:, c:c + 1],
                                                              in1=invB[:], op0=ALU.mult, op1=ALU.mult),
             reads=(("xst", i), "invB", "gmix"), writes=(("hT", c),))
    hT_all = tuple(("hT", c) for c in range(16))
    if stage == 1:
        dump("hT", hT[:], [128, 16, 2048], BF16, hT_all)
        return finish()

    zf = sb("zf", [128, 16, 512], BF16, 108.5)
    qT = sb("qT", [128, 4, 2048], BF16, 124.5)
    yapT = sb("yapT", [128, 8, 2048], BF16, 140.5)
    pooled = sb("pooled", [128, 2, 2048], BF16, 108.5)
    LP = 2048 + 32
    upad = sb("upad", [128, LP], F32, T0)
    tA = sb("tA", [128, LP], F32, T0 + 8.25)
    tB = sb("tB", [128, LP], F32, T0 + 16.5)
    invc = sb("invc", [128, 2048], F32, T0 + 24.75)
    pw_sb = sb("pw_sb", [128, 2, 256], BF16, 205.5)
    win_d = inp("w_in", [2048, 8192]).rearrange("(c p) n -> p c n", p=128)
    invc_d = inp("cst_invc", [4, 128, 2048])
    poolw_d = inp("pool_w", [1024, 256]).rearrange("(g k p) n -> g p k n", k=2, p=128)

    p0dead = (("xst", 0), ("xst", 1), ("sq", 0), ("sq", 1), "invB")
    P.alias(["upad", "tA", "tB", "invc", "pw_sb"], p0dead)
    P.op("pool", lambda e: e.memset(upad[:], 0.0), reads=(), writes=("upad",))
    P.op("pool", lambda e: e.memset(tA[:], 0.0), reads=(), writes=("tA",))
    P.op("pool", lambda e: e.memset(tB[:], 0.0), reads=(), writes=("tB",))

    for cb in range(8):
        s = next_slot()
        P.dma("pool", "ws%d" % s, lambda e, s=s, cb=cb: e.dma_start(out=wslot[s][:], in_=win_d[:, :, cb * 256:(cb + 1) * 256]),
              writes=(("ws", s),))
        if cb < 4:
            g = cb
            P.dma("sp", "invc", lambda e, g=g: e.dma_start(out=invc[:], in_=invc_d[g]), writes=("invc",))
            P.dma("pool", "pw", lambda e, g=g: e.dma_start(out=pw_sb[:], in_=poolw_d[g]), writes=("pw_sb",))
            for pc in range(2):
                for tb in range(4):
                    b = next_bank()
                    for c in range(16):
                        mm(bank(b), wslot[s][:, c, pc * 128:(pc + 1) * 128], hT[:, c, tb * 512:(tb + 1) * 512], c == 0, c == 15,
                           reads=(("ws", s), ("hT", c)), writes=(("ps", b),))
                    P.op("act", lambda e, b=b, tb=tb: e.copy(out=upad[:, 16 + tb * 512:16 + (tb + 1) * 512], in_=bank(b)),
                         reads=(("ps", b),), writes=("upad",))
                lvl = g + 1
                src = upad
                bufs = [tA, tB]
                sh = [(1, 0), (1, -1), (2, -2), (4, -4)]
                specs = [(1, 0, 1, LP), (1, 1, 2, LP - 1), (2, 2, 4, LP - 3), (4, 4, 8, LP - 7)]
                cur = upad
                curname = "upad"
                for l in range(lvl):
                    dl, dr, lo, hi = specs[l]
                    dst = bufs[l % 2]
                    dname = "tA" if l % 2 == 0 else "tB"
                    P.op("dve", lambda e, dst=dst, cur=cur, dl=dl, dr=dr, lo=lo, hi=hi:
                         e.tensor_tensor(out=dst[:, lo:hi], in0=cur[:, lo - dl:hi - dl], in1=cur[:, lo + dr:hi + dr], op=ALU.add),
                         reads=(curname,), writes=(dname,))
                    cur, curname = dst, dname
                oth = bufs[lvl % 2]
                oname = "tA" if lvl % 2 == 0 else "tB"
                P.op("dve", lambda e, cur=cur, oth=oth: e.tensor_tensor(out=oth[:, 16:16 + 2048], in0=cur[:, 16:16 + 2048],
                                                                       in1=invc[:], op=ALU.mult),
                     reads=(curname, "invc"), writes=(oname,))
                P.op("dve", lambda e, oth=oth, pc=pc: e.tensor_tensor(out=pooled[:, pc, :], in0=oth[:, 16:16 + 2048],
                                                                     in1=upad[:, 16:16 + 2048], op=ALU.subtract),
                     reads=(oname, "upad"), writes=(("pooled", pc),))
            for oc in range(2):
                for tb in range(4):
                    b = next_bank()
                    for kc in range(2):
                        mm(bank(b), pw_sb[:, kc, oc * 128:(oc + 1) * 128], pooled[:, kc, tb * 512:(tb + 1) * 512], kc == 0, kc == 1,
                           reads=("pw_sb", ("pooled", 0), ("pooled", 1)), writes=(("ps", b),))
                    ch = 2 * g + oc
                    evac(yapT[:, ch, tb * 512:(tb + 1) * 512], bank(b), reads=(("ps", b), "pscale"), writes=(("yapT", ch),),
                         scale=pscale[:, ch:ch + 1])
        elif cb < 6:
            if cb == 4:
                P.alias([("zf", t) for t in range(16)], [("pooled", 0), ("pooled", 1)])
            for tt in range(16):
                if tt % 2 == 0:
                    b = next_bank()
                half = (tt % 2) * 256
                for c in range(16):
                    mm(bank(b)[:, half:half + 256], hT[:, c, tt * 128:(tt + 1) * 128], wslot[s][:, c, :], c == 0, c == 15,
                       reads=(("ws", s), ("hT", c)), writes=(("ps", b),))
                evac(zf[:, tt, (cb - 4) * 256:(cb - 3) * 256], bank(b)[:, half:half + 256], reads=(("ps", b),),
                     writes=(("zf", tt),))
        else:
            for hh in range(2):
                h = 2 * (cb - 6) + hh
                for tb in range(4):
                    b = next_bank()
                    for c in range(16):
                        mm(bank(b), wslot[s][:, c, hh * 128:(hh + 1) * 128], hT[:, c, tb * 512:(tb + 1) * 512], c == 0, c == 15,
                           reads=(("ws", s), ("hT", c)), writes=(("ps", b),))
                    evac(qT[:, h, tb * 512:(tb + 1) * 512], bank(b), reads=(("ps", b),), writes=(("qT", h),))
    yapT_all = tuple(("yapT", c) for c in range(8))
    zf_all = tuple(("zf", t) for t in range(16))
    if stage == 2:
        dump("yapT", yapT[:], [128, 8, 2048], BF16, yapT_all)
        dump("zf", zf[:], [128, 16, 512], BF16, zf_all)
        dump("qT", qT[:], [128, 4, 2048], BF16, tuple(("qT", h) for h in range(4)))
        return finish()

    ocT = sb("ocT", [128, 4, 2048], BF16, T0)
    expT = [sb("expT%d" % i, [128, 2, 512], BF16, T0 + 16 + 2 * i) for i in range(2)]
    rden = [sb("rden%d" % i, [128, 512], F32, T0 + 20 + 2 * i) for i in range(2)]
    pooldead = ("upad", "tA", "tB", "invc")
    P.alias([("expT", 0), ("expT", 1), ("rden", 0), ("rden", 1)] + [("ocT", h) for h in range(4)], pooldead)
    it = 0
    for h in range(4):
        for tb in range(4):
            i = it % 2
            it += 1
            for mc in range(2):
                b = next_bank()
                mm(bank(b), kT[:, h, mc * 128:(mc + 1) * 128], qT[:, h, tb * 512:(tb + 1) * 512], True, True,
                   reads=("kT", ("qT", h)), writes=(("ps", b),))
                P.op("act", lambda e, b=b, i=i, mc=mc: e.activation(out=expT[i][:, mc, :], in_=bank(b), func=AF.Exp,
                                                                   scale=float(128 ** -0.5)),
                     reads=(("ps", b),), writes=(("expT", i),))
            bd = next_bank()
            for mc in range(2):
                mm(bank(bd), ones_bf[:], expT[i][:, mc, :], mc == 0, mc == 1, reads=(("expT", i), "ones_bf"), writes=(("ps", bd),))
            bo = next_bank()
            for mc in range(2):
                mm(bank(bo), vv[:, mc, h * 128:(h + 1) * 128], expT[i][:, mc, :], mc == 0, mc == 1,
                   reads=(("expT", i), "vv"), writes=(("ps", bo),))
            P.op("dve", lambda e, bd=bd, i=i: e.reciprocal(out=rden[i][:], in_=bank(bd)), reads=(("ps", bd),),
                 writes=(("rden", i),))
            P.op("dve", lambda e, bo=bo, i=i, h=h, tb=tb: e.tensor_tensor(out=ocT[:, h, tb * 512:(tb + 1) * 512], in0=bank(bo),
                                                                         in1=rden[i][:], op=ALU.mult),
                 reads=(("ps", bo), ("rden", i)), writes=(("ocT", h),))
    ocT_all = tuple(("ocT", h) for h in range(4))
    if stage == 3:
        dump("ocT", ocT[:], [128, 4, 2048], BF16, ocT_all)
        return finish()

    ybT = sb("ybT", [128, 4, 2048], BF16, T0 + 16)
    fw_sb = sb("fw_sb", [128, 4, 128], BF16, 124.5)
    Wc = sb("Wc", [128, 4, 128], BF16, 125.5)
    Wsn = sb("Wsn", [128, 4, 128], BF16, 126.5)
    T1sb = [sb("T1sb%d" % i, [128, 512], BF16, 127.5 + i) for i in range(4)]
    T2sb = [sb("T2sb%d" % i, [128, 512], BF16, 131.5 + i) for i in range(4)]
    qdead = tuple(("qT", h) for h in range(4))
    attdead = (("expT", 0), ("expT", 1), ("rden", 0), ("rden", 1))
    fw_d = inp("fourier_w", [512, 128]).rearrange("(g p) n -> p g n", p=128)
    cosS_d = inp("cst_cosS", [2048, 2048]).rearrange("(c p) k -> p c k", p=128)
    sinS_d = inp("cst_sinS", [2048, 2048]).rearrange("(c p) k -> p c k", p=128)
    P.alias(["fw_sb", "Wc", "Wsn"] + [("T1sb", i) for i in range(4)] + [("T2sb", i) for i in range(4)], qdead)
    P.alias([("ybT", g) for g in range(4)], attdead)
    P.dma("pool", "fw", lambda e: e.dma_start(out=fw_sb[:], in_=fw_d), writes=("fw_sb",))
    for g in range(4):
        for (cm, dstt, nm) in ((c128_bf, Wc, "Wc"), (ns128_bf, Wsn, "Wsn")):
            b = next_bank()
            mm(bank(b)[:, 0:128], cm[:], fw_sb[:, g, :], True, True, reads=("fw_sb", cm.name), writes=(("ps", b),))
            evac(dstt[:, g, :], bank(b)[:, 0:128], reads=(("ps", b),), writes=(nm,))
    wslot8 = [sb("wslot8_%d" % i, [128, 8, 512], BF16, 64 + 8 * i) for i in range(4)]
    for kb in range(4):
        for (nm, src, dst, dn) in (("c", cosS_d, T1sb, "T1sb"), ("s", sinS_d, T2sb, "T2sb")):
            sl = []
            for hf in range(2):
                s = next_slot()
                sl.append(s)
                P.dma("pool", "ws%d" % s, lambda e, s=s, src=src, hf=hf, kb=kb: e.dma_start(
                    out=wslot8[s][:], in_=src[:, hf * 8:(hf + 1) * 8, kb * 512:(kb + 1) * 512]), writes=(("ws", s),))
            for g in range(4):
                b = next_bank()
                for sc in range(16):
                    s = sl[sc // 8]
                    mm(bank(b), zf[:, sc, g * 128:(g + 1) * 128], wslot8[s][:, sc % 8, :], sc == 0, sc == 15,
                       reads=(("ws", s), ("zf", sc)), writes=(("ps", b),))
                evac(dst[g][:], bank(b), reads=(("ps", b),), writes=((dn, g),))
        for g in range(4):
            b = next_bank()
            mm(bank(b), Wc[:, g, :], T1sb[g][:], True, False, reads=("Wc", ("T1sb", g)), writes=(("ps", b),))
            mm(bank(b), Wsn[:, g, :], T2sb[g][:], False, True, reads=("Wsn", ("T2sb", g)), writes=(("ps", b),))
            evac(ybT[:, g, kb * 512:(kb + 1) * 512], bank(b), reads=(("ps", b),), writes=(("ybT", g),))
    ybT_all = tuple(("ybT", g) for g in range(4))
    if stage == 4:
        dump("ybT", ybT[:], [128, 4, 2048], BF16, ybT_all)
        return finish()

    gbuf = [sb("gbuf%d" % i, [128, 3, 16, 128], BF16, 64 + 16 * i) for i in range(2)]
    ppb = [sb("ppb%d" % i, [128, 8, 128], BF16, 64 + 16 * i + 12) for i in range(2)]
    pfb = [sb("pfb%d" % i, [128, 4, 128], BF16, 64 + 16 * i + 14) for i in range(2)]
    pmb = [sb("pmb%d" % i, [128, 4, 128], BF16, 64 + 16 * i + 15) for i in range(2)]
    sg = [[sb("sg%d_%d" % (i, j), [128, 512], F32, 108.5 + 6 * i + 2 * j) for j in range(3)] for i in range(2)]
    mch = [sb("mch%d" % i, [128, 2048], BF16, 120.5 + 4 * i) for i in range(2)]
    pp_d = inp("proj_pool", [1024, 2048]).rearrange("(c p) n -> p c n", p=128)
    pf_d = inp("proj_fourier", [512, 2048]).rearrange("(c p) n -> p c n", p=128)
    pm_d = inp("proj_mem", [512, 2048]).rearrange("(c p) n -> p c n", p=128)
    mT_d = nc.dram_tensor("mT_scr", [16, 128, 2048], BF16, kind="Internal").ap()
    fdead = zf_all + ("fw_sb", "Wc", "Wsn") + tuple(("T1sb", i) for i in range(4)) + tuple(("T2sb", i) for i in range(4)) + qdead
    P.alias([("sg", i, j) for i in range(2) for j in range(3)] + [("mch", 0), ("mch", 1)], fdead)
    it = 0
    for dc in range(16):
        wb = dc % 2
        wkeys = (("ws", 2 * wb), ("ws", 2 * wb + 1))
        for j in range(3):
            P.dma("pool", "cg%d_%d" % (wb, j), lambda e, wb=wb, j=j, dc=dc: e.dma_start(
                out=gbuf[wb][:, j, :, :], in_=win_d[:, :, 2048 + j * 2048 + dc * 128:2048 + j * 2048 + (dc + 1) * 128]),
                writes=wkeys)
        P.dma("pool", "cpp%d" % wb, lambda e, wb=wb, dc=dc: e.dma_start(out=ppb[wb][:], in_=pp_d[:, :, dc * 128:(dc + 1) * 128]), writes=wkeys)
        P.dma("pool", "cpf%d" % wb, lambda e, wb=wb, dc=dc: e.dma_start(out=pfb[wb][:], in_=pf_d[:, :, dc * 128:(dc + 1) * 128]), writes=wkeys)
        P.dma("pool", "cpm%d" % wb, lambda e, wb=wb, dc=dc: e.dma_start(out=pmb[wb][:], in_=pm_d[:, :, dc * 128:(dc + 1) * 128]), writes=wkeys)
        mi = dc % 2
        for tb in range(4):
            i = it % 2
            it += 1
            tsl = slice(tb * 512, (tb + 1) * 512)
            ybanks = []
            for (wt, src, nk, rk) in ((ppb[wb], yapT, 8, yapT_all), (pfb[wb], ybT, 4, ybT_all), (pmb[wb], ocT, 4, ocT_all)):
                b = next_bank()
                ybanks.append(b)
                for kc in range(nk):
                    mm(bank(b), wt[:, kc, :], src[:, kc, tsl], kc == 0, kc == nk - 1, reads=wkeys + rk, writes=(("ps", b),))
            for j in range(3):
                b = next_bank()
                for c in range(16):
                    mm(bank(b), gbuf[wb][:, j, c, :], hT[:, c, tsl], c == 0, c == 15, reads=wkeys + (("hT", c),), writes=(("ps", b),))
                P.op("act", lambda e, b=b, i=i, j=j: e.activation(out=sg[i][j][:], in_=bank(b), func=AF.Sigmoid),
                     reads=(("ps", b),), writes=(("sg", i, j),))
            for j in range(3):
                P.op("dve", lambda e, i=i, j=j, yb=ybanks[j]: e.tensor_tensor(out=sg[i][j][:], in0=sg[i][j][:], in1=bank(yb), op=ALU.mult),
                     reads=(("sg", i, j), ("ps", ybanks[j])), writes=(("sg", i, j),))
            P.op("dve", lambda e, i=i: e.tensor_tensor(out=sg[i][0][:], in0=sg[i][0][:], in1=sg[i][1][:], op=ALU.add),
                 reads=(("sg", i, 0), ("sg", i, 1)), writes=(("sg", i, 0),))
            P.op("dve", lambda e, i=i, mi=mi, tsl=tsl: e.tensor_tensor(out=mch[mi][:, tsl], in0=sg[i][0][:], in1=sg[i][2][:], op=ALU.add),
                 reads=(("sg", i, 0), ("sg", i, 2)), writes=(("mch", mi),))
        P.dma("sp", "mspill%d" % mi, lambda e, mi=mi, dc=dc: e.dma_start(out=mT_d[dc], in_=mch[mi][:]),
              reads=(("mch", mi),), writes=("mT_d",))
    if stage == 5:
        o = nc.dram_tensor("dbg_mT", [16, 128, 2048], BF16, kind="ExternalOutput").ap()
        stg = sb("dbgstg", [128, 16, 2048], BF16, 0)
        l1 = P.dma("sp", "dbgl", lambda e: e.dma_start(out=stg[:], in_=mT_d.rearrange("c p t -> p c t")), reads=("mT_d",) + hT_all,
                   writes=hT_all)
        final_ops.append(P.dma("sp", "dbgs", lambda e: e.dma_start(out=o.rearrange("c p t -> p c t"), in_=stg[:]), reads=hT_all))
        return finish()

    wout = sb("wout", [128, 16, 2048], BF16, 0)
    mTb = [sb("mTb%d" % i, [128, 16, 512], BF16, 64 + 16 * i) for i in range(2)]
    h2 = sb("h2", [128, 16, 2048], BF16, 104.5)
    xt = [sb("xt%d" % i, [128, 2048], F32, 168.5 + 8 * i) for i in range(2)]
    gffn = sb("gffn", [128, 2048], F32, 184.5)
    h2T = [sb("h2T%d" % i, [128, 16, 128], BF16, 192.5 + 4 * i) for i in range(2)]
    junk = sb("junk", [128, 2048], BF16, 200.5)
    lg = sb("lg", [128, 16], F32, 204.5)
    wout_d = inp("w_out", [2048, 2048]).rearrange("(c p) n -> p c n", p=128)
    x_d = inp("x", [2048, 2048])
    gffn_d = inp("gffn_rep", [128, 2048])
    wr_d = inp("w_router", [2048, 16]).rearrange("(c p) n -> p c n", p=128)
    x1_d = nc.dram_tensor("x1_scr", [2048, 2048], F32, kind="Internal").ap()
    cdead = (tuple(("sg", i, j) for i in range(2) for j in range(3)) + (("mch", 0), ("mch", 1)) + yapT_all + ybT_all + ocT_all
             + ("kT", "vv", "pw_sb"))
    allws = tuple(("ws", s) for s in range(4))
    P.alias([("wout", blk) for blk in range(8)], hT_all)
    P.alias([("xt", 0), ("xt", 1), "gffn", ("h2T", 0), ("h2T", 1), "junk", "lg"] + [("h2", t) for t in range(16)], cdead)
    for blk in range(8):
        P.dma("pool", "wout%d" % (blk % 4), lambda e, blk=blk: e.dma_start(out=wout[:, :, blk * 256:(blk + 1) * 256],
                                                                         in_=wout_d[:, :, blk * 256:(blk + 1) * 256]),
              reads=(), writes=(("wout", blk),))
    wout_all = tuple(("wout", blk) for blk in range(8))
    P.dma("sp", "gffn", lambda e: e.dma_start(out=gffn[:], in_=gffn_d), writes=("gffn",))
    P.dma("pool", "wr", lambda e: e.dma_start(out=wr_bf[:], in_=wr_d), writes=("wr_bf",))
    mT_v = mT_d.rearrange("c p t -> p c t")
    SSQ, INV2, MX, SM = 0, 16, 32, 48
    for tg in range(4):
        mb = tg % 2
        P.dma("sp", "mTb%d" % mb, lambda e, mb=mb, tg=tg: e.dma_start(out=mTb[mb][:], in_=mT_v[:, :, tg * 512:(tg + 1) * 512]),
              reads=("mT_d",), writes=(("ws", 2 * mb), ("ws", 2 * mb + 1)))
        for tt in range(4):
            ti = tg * 4 + tt
            xi = ti % 2
            P.dma("sp", "xt%d" % xi, lambda e, xi=xi, ti=ti: e.dma_start(out=xt[xi][:], in_=x_d[ti * 128:(ti + 1) * 128, :]),
                  writes=(("xt", xi),))
            for db in range(4):
                b = next_bank()
                for c in range(16):
                    mm(bank(b), mTb[mb][:, c, tt * 128:(tt + 1) * 128], wout[:, c, db * 512:(db + 1) * 512], c == 0, c == 15,
                       reads=(("ws", 2 * mb), ("ws", 2 * mb + 1), ("wout", 2 * db), ("wout", 2 * db + 1)), writes=(("ps", b),))
                P.op("dve", lambda e, b=b, xi=xi, db=db: e.tensor_tensor(out=xt[xi][:, db * 512:(db + 1) * 512],
                                                                        in0=xt[xi][:, db * 512:(db + 1) * 512], in1=bank(b), op=ALU.add),
                     reads=(("ps", b), ("xt", xi)), writes=(("xt", xi),))
            P.dma("sp", "x1sp%d" % xi, lambda e, xi=xi, ti=ti: e.dma_start(out=x1_d[ti * 128:(ti + 1) * 128, :], in_=xt[xi][:]),
                  reads=(("xt", xi),), writes=("x1_d",))
            P.op("act", lambda e, xi=xi, ti=ti: e.activation(out=junk[:], in_=xt[xi][:], func=AF.Square,
                                                            accum_out=small[:, SSQ + ti:SSQ + ti + 1]),
                 reads=(("xt", xi),), writes=("junk", ("ssq", ti)))
            P.op("act", lambda e, ti=ti: e.activation(out=small[:, INV2 + ti:INV2 + ti + 1], in_=small[:, SSQ + ti:SSQ + ti + 1],
                                                     func=AF.Sqrt, bias=EPS, scale=1.0 / D),
                 reads=(("ssq", ti),), writes=(("inv2", ti),))
            P.op("dve", lambda e, ti=ti: e.reciprocal(out=small[:, INV2 + ti:INV2 + ti + 1], in_=small[:, INV2 + ti:INV2 + ti + 1]),
                 reads=(("inv2", ti),), writes=(("inv2", ti),))
            P.op("dve", lambda e, xi=xi, ti=ti: e.scalar_tensor_tensor(out=h2[:, ti, :], in0=xt[xi][:], scalar=small[:, INV2 + ti:INV2 + ti + 1],
                                                                      in1=gffn[:], op0=ALU.mult, op1=ALU.mult),
                 reads=(("xt", xi), ("inv2", ti), "gffn"), writes=(("h2", ti),))
            hi_ = ti % 2
            for half in range(2):
                b = next_bank()
                for cc in range(8):
                    c = half * 8 + cc
                    P.op("pe", lambda e, b=b, cc=cc, c=c, ti=ti: e.transpose(bank_bf(b)[:, cc * 128:(cc + 1) * 128],
                                                                             h2[:, ti, c * 128:(c + 1) * 128], ident_bf[:]),
                         reads=(("h2", ti), "ident_bf"), writes=(("ps", b),))
                evac(h2T[hi_][:, half * 8:(half + 1) * 8, :].rearrange("p c t -> p (c t)"), bank_bf(b), reads=(("ps", b),),
                     writes=(("h2T", hi_),))
            b = next_bank()
            for c in range(16):
                mm(bank(b)[:, 0:16], h2T[hi_][:, c, :], wr_bf[:, c, :], c == 0, c == 15, reads=(("h2T", hi_), "wr_bf"), writes=(("ps", b),))
            P.op("dve", lambda e, b=b, ti=ti: e.reduce_max(out=small[:, MX + ti:MX + ti + 1], in_=bank(b)[:, 0:16], axis=AX.X),
                 reads=(("ps", b),), writes=(("mx", ti),))
            P.op("dve", lambda e, ti=ti: e.tensor_scalar(out=small[:, MX + ti:MX + ti + 1], in0=small[:, MX + ti:MX + ti + 1],
                                                        scalar1=-1.0, scalar2=None, op0=ALU.mult),
                 reads=(("mx", ti),), writes=(("mx", ti),))
            P.op("act", lambda e, b=b, ti=ti: e.activation(out=lg[:], in_=bank(b)[:, 0:16], func=AF.Exp,
                                                          bias=small[:, MX + ti:MX + ti + 1], scale=1.0,
                                                          accum_out=small[:, SM + ti:SM + ti + 1]),
                 reads=(("ps", b), ("mx", ti)), writes=("lg", ("sm", ti)))
            P.op("dve", lambda e, ti=ti: e.reciprocal(out=small[:, SM + ti:SM + ti + 1], in_=small[:, SM + ti:SM + ti + 1]),
                 reads=(("sm", ti),), writes=(("sm", ti),))
            P.op("dve", lambda e, ti=ti: e.tensor_scalar(out=aff_tm[:, ti, :], in0=lg[:], scalar1=small[:, SM + ti:SM + ti + 1],
                                                        scalar2=None, op0=ALU.mult),
                 reads=("lg", ("sm", ti)), writes=("aff_tm",))
    h2_all = tuple(("h2", t) for t in range(16))
    if stage == 6:
        dump("h2", h2[:], [128, 16, 2048], BF16, h2_all)
        dump("aff", aff_tm[:], [128, 16, 16], F32, ("aff_tm",))
        return finish()

    affT = sb("affT", [16, 2048], F32, 168.5)
    work = sb("work", [16, 2048], F32, 176.5)
    maskT = sb("maskT", [128, 2048], BF16, 184.5)
    mx8 = sb("mx8", [16, 8], F32, 188.5)
    selT = [sb("selT%d" % i, [128, 16, 512], BF16, 0 + 16 * i) for i in range(2)]
    xin_st = [sb("xin_st%d" % i, [128, 16, 512], BF16, 32 + 16 * i) for i in range(2)]
    xin_d = nc.dram_tensor("xin_scr", [8, 128, 16, 512], BF16, kind="Internal").ap()
    ddead = (("xt", 0), ("xt", 1), "gffn", ("h2T", 0), ("h2T", 1), "junk", "lg")
    P.alias(["affT", "work", "maskT", "mx8"], ddead)
    P.alias([("selT", 0), ("selT", 1), ("xin_st", 0), ("xin_st", 1)], wout_all)
    aff3 = sb("aff3", [128, 3, 16, 32], BF16, 189.0)
    rtmp = sb("rtmp", [128, 16, 16], F32, 192.0)
    P.alias(["aff3", "rtmp"], ddead)
    P.op("pool", lambda e: e.memset(aff3[:], 0.0), writes=("aff3",))
    P.op("dve", lambda e: e.tensor_copy(out=aff3[:, 0, :, 0:16], in_=aff_tm[:]), reads=("aff_tm",), writes=("aff3",))
    P.op("dve", lambda e: e.tensor_tensor(out=rtmp[:], in0=aff_tm[:], in1=aff3[:, 0, :, 0:16], op=ALU.subtract), reads=("aff_tm", "aff3"), writes=("rtmp",))
    P.op("dve", lambda e: e.tensor_copy(out=aff3[:, 1, :, 0:16], in_=rtmp[:]), reads=("rtmp",), writes=("aff3",))
    P.op("dve", lambda e: e.tensor_tensor(out=rtmp[:], in0=rtmp[:], in1=aff3[:, 1, :, 0:16], op=ALU.subtract), reads=("rtmp", "aff3"), writes=("rtmp",))
    P.op("dve", lambda e: e.tensor_copy(out=aff3[:, 2, :, 0:16], in_=rtmp[:]), reads=("rtmp",), writes=("aff3",))
    if stage == 7.05:
        dump("aff3", aff3[:], [128, 3, 16, 32], BF16, ("aff3",))
        return finish()
    bks = [next_bank() for _ in range(4)]
    for ti in range(16):
        b = bks[ti // 4]
        off = (ti % 4) * 128
        for k3 in range(3):
            mm(bank(b)[0:32, off:off + 128], aff3[:, k3, ti, :], ident_bf[:], k3 == 0, k3 == 2,
               reads=("aff3", "ident_bf"), writes=(("ps", b),))
    for q in range(4):
        P.op("act", lambda e, q=q, bks=bks: e.copy(out=affT[:, q * 512:(q + 1) * 512], in_=bank(bks[q])[0:16, :]),
             reads=(("ps", bks[q]),), writes=("affT",))
    P.op("dve", lambda e: e.tensor_copy(out=work[:], in_=affT[:]), reads=("affT",), writes=("work",))
    if stage == 7.1:
        dump("affT", affT[:], [16, 2048], F32, ("affT",))
        return finish()
    LO, TT, CNT, FLG = 0, 1, 2, 3
    P.op("dve", lambda e: e.memset(mx8[:], 0.0), writes=("mx8",))
    for kbit in range(26):
        step = float(2.0 ** -(kbit + 1))
        P.op("dve", lambda e, step=step: e.tensor_scalar(out=work[:], in0=affT[:], scalar1=mx8[:, LO:LO + 1], scalar2=step,
                                                        op0=ALU.subtract, op1=ALU.is_ge),
             reads=("mx8", "affT"), writes=("work",))
        P.op("dve", lambda e: e.reduce_sum(out=mx8[:, CNT:CNT + 1], in_=work[:], axis=AX.X), reads=("work",), writes=("mx8",))
        P.op("dve", lambda e, step=step: e.tensor_scalar(out=mx8[:, FLG:FLG + 1], in0=mx8[:, CNT:CNT + 1], scalar1=float(CAP) - 0.5,
                                                        scalar2=step, op0=ALU.is_ge, op1=ALU.mult),
             reads=("mx8",), writes=("mx8",))
        P.op("dve", lambda e: e.tensor_tensor(out=mx8[:, LO:LO + 1], in0=mx8[:, LO:LO + 1], in1=mx8[:, FLG:FLG + 1], op=ALU.add),
             reads=("mx8",), writes=("mx8",))
    if stage == 7.2:
        dump("mx8", mx8[:], [16, 8], F32, ("mx8",))
        return finish()
    P.op("pool", lambda e: e.memset(maskT[:], 0.0), writes=("maskT",))
    P.op("dve", lambda e: e.tensor_scalar(out=maskT[0:16, :], in0=affT[:], scalar1=mx8[:, 0:1], scalar2=None, op0=ALU.is_ge),
         reads=("affT", "mx8"), writes=("maskT",))
    if stage == 7.3:
        dump("maskT", maskT[0:16, :], [16, 2048], BF16, ("maskT",))
        return finish()
    b = next_bank()
    for ti in range(16):
        mm(bank(b)[:, ti * 16:(ti + 1) * 16], maskT[:, ti * 128:(ti + 1) * 128], ident_bf[:, 0:16], True, True,
           reads=("maskT", "ident_bf"), writes=(("ps", b),))
    P.op("act", lambda e, b=b: e.copy(out=mask_tm[:].rearrange("p a b -> p (a b)"), in_=bank(b)[:, 0:256]),
         reads=(("ps", b),), writes=("mask_tm",))
    if stage == 7.35:
        dump("mask", mask_tm[:], [128, 16, 16], F32, ("mask_tm",))
        return finish()
    P.op("dve", lambda e: e.tensor_copy(out=maskbf_tm[:], in_=mask_tm[:]), reads=("mask_tm",), writes=("maskbf_tm",))
    if stage == 7.4:
        dump("mask", mask_tm[:], [128, 16, 16], F32, ("mask_tm",))
        return finish()
    P.op("dve", lambda e: e.tensor_tensor(out=A_tm[:].rearrange("p a b -> p (a b)"), in0=mask_tm[:].rearrange("p a b -> p (a b)"),
                                          in1=aff_tm[:].rearrange("p a b -> p (a b)"), op=ALU.mult),
         reads=("mask_tm", "aff_tm"), writes=("A_tm",))
    b = next_bank()
    for ti in range(16):
        for j in range(ti + 1):
            lhs = tri_bf if j == ti else ones_bf
            mm(bank(b)[:, ti * 16:(ti + 1) * 16], lhs[:], maskbf_tm[:, j, :], j == 0, j == ti,
               reads=("maskbf_tm", "tri_bf", "ones_bf"), writes=(("ps", b),))
    P.op("act", lambda e, b=b: e.copy(out=pos_tm[:].rearrange("p a b -> p (a b)"), in_=bank(b)[:, 0:256]),
         reads=(("ps", b),), writes=("pos_tm",))
    if stage == 7:
        dump("mask", mask_tm[:], [128, 16, 16], F32, ("mask_tm",))
        dump("pos", pos_tm[:], [128, 16, 16], F32, ("pos_tm",))
        dump("A", A_tm[:], [128, 16, 16], F32, ("A_tm",))
        return finish()

    def onehot(ep):
        si = ep % 2
        for ti in range(16):
            for ee in range(2):
                e_ = 2 * ep + ee
                P.op("dve", lambda e, si=si, ti=ti, ee=ee, e_=e_: e.tensor_scalar(
                    out=selT[si][:, ti, ee * 256:(ee + 1) * 256], in0=iota_f[:], scalar1=pos_tm[:, ti, e_:e_ + 1],
                    scalar2=mask_tm[:, ti, e_:e_ + 1], op0=ALU.is_equal, op1=ALU.mult),
                    reads=("iota_f", "pos_tm", "mask_tm"), writes=(("selT", si),))

    onehot(0)
    for ep in range(8):
        si = ep % 2
        if ep + 1 < 8:
            onehot(ep + 1)
        for c in range(16):
            b = next_bank()
            for ti in range(16):
                mm(bank(b), h2[:, ti, c * 128:(c + 1) * 128], selT[si][:, ti, :], ti == 0, ti == 15,
                   reads=(("selT", si), ("h2", ti)), writes=(("ps", b),))
            P.op("act", lambda e, si=si, c=c, b=b: e.copy(out=xin_st[si][:, c, :], in_=bank(b)), reads=(("ps", b),),
                 writes=(("xin_st", si),))
        P.dma("sp", "xinsp%d" % si, lambda e, si=si, ep=ep: e.dma_start(out=xin_d[ep], in_=xin_st[si][:]),
              reads=(("xin_st", si),), writes=(("xin_d", ep),))
    if stage == 8:
        o = nc.dram_tensor("dbg_xin", [8, 128, 16, 512], BF16, kind="ExternalOutput").ap()
        for ep in range(8):
            si = ep % 2
            P.dma("sp", "dbgl%d" % si, lambda e, si=si, ep=ep: e.dma_start(out=xin_st[si][:], in_=xin_d[ep]),
                  reads=(("xin_d", ep),), writes=(("xin_st", si),))
            final_ops.append(P.dma("sp", "dbgs%d" % si, lambda e, si=si, ep=ep: e.dma_start(out=o[ep], in_=xin_st[si][:]),
                                   reads=(("xin_st", si),)))
        return finish()

    accA = sb("accA", [128, 8, 2048], F32, 0)
    accB = sb("accB", [128, 8, 2048], F32, 104.5)

    def acc(ti):
        return (accA if ti < 8 else accB)[:, ti % 8, :]

    xinT = sb("xinT", [128, 16, 256], BF16, 168.5)
    hidT = [sb("hidT%d" % i, [128, 16, 256], BF16, 176.5 + 8 * i) for i in range(2)]
    selA = sb("selA", [128, 2, 2048], BF16, 192.5)
    selAT = [sb("selAT0", [128, 4, 256], BF16, 200.5)]
    sil4 = sb("sil4", [128, 4, 256], F32, 202.5)
    ysb = sb("ysb", [128, 2, 512], BF16, CK + 3.5)
    wd8 = [sb("wd8_%d" % i, [128, 8, 512], BF16, 64 + 8 * i) for i in range(4)]
    wg_d = inp("w_gate", [16, 2048, 2048])
    wu_d = inp("w_up", [16, 2048, 2048])
    wd_d = inp("w_down", [16, 2048, 2048])
    e0dead = (("selT", 0), ("selT", 1), ("xin_st", 0), ("xin_st", 1)) + h2_all + ("affT", "work", "maskT", "mx8")
    P.alias([("acc", t) for t in range(16)] + ["xinT", ("hidT", 0), ("hidT", 1), "selA", ("selAT", 0)]
            + [("sil", f4) for f4 in range(4)], e0dead + ddead)
    P.alias([("ysb", 0), ("ysb", 1)], ["aff_tm", "mask_tm"])
    P.dma("sp", "xinld", lambda e: e.dma_start(out=xinT[:], in_=xin_d[0][:, :, 0:256]), reads=(("xin_d", 0),), writes=("xinT",))
    for ti in range(16):
        P.dma("sp", "accld%d" % (ti % 4), lambda e, ti=ti: e.dma_start(out=acc(ti), in_=x1_d[ti * 128:(ti + 1) * 128, :]),
              reads=("x1_d",), writes=(("acc", ti),))
    wviews = {}

    def wv(ex):
        if ex not in wviews:
            wviews[ex] = (wg_d[ex].rearrange("(c p) n -> p c n", p=128), wu_d[ex].rearrange("(c p) n -> p c n", p=128),
                          wd_d[ex].rearrange("(c p) n -> p c n", p=128))
        return wviews[ex]

    def xin_load(ex):
        ep, ee = ex // 2, ex % 2
        P.dma("sp", "xinld", lambda e, ep=ep, ee=ee: e.dma_start(out=xinT[:], in_=xin_d[ep][:, :, ee * 256:(ee + 1) * 256]),
              reads=(("xin_d", ep),), writes=("xinT",))

    def gate_up_step(ex, q):
        wgv, wuv, _ = wv(ex)
        hi_ = ex % 2
        for (wview, kind) in ((wgv, "g"), (wuv, "u")):
            sl = []
            for hf in range(2):
                s = next_slot()
                sl.append(s)
                P.dma("pool", "ws%d" % s, lambda e, s=s, hf=hf, q=q, wview=wview: e.dma_start(
                    out=wd8[s][:], in_=wview[:, hf * 8:(hf + 1) * 8, q * 512:(q + 1) * 512]), writes=(("ws", s),))
            banks = [next_bank(), next_bank()]
            for hf in range(2):
                s = sl[hf]
                for f4 in range(4):
                    b = banks[f4 // 2]
                    off = (f4 % 2) * 256
                    for cc in range(8):
                        c = hf * 8 + cc
                        mm(bank(b)[:, off:off + 256], wd8[s][:, cc, f4 * 128:(f4 + 1) * 128], xinT[:, c, :], c == 0 and f4 % 2 == 0, c == 15,
                           reads=(("ws", s), "xinT"), writes=(("ps", b),))
            for f4 in range(4):
                b = banks[f4 // 2]
                off = (f4 % 2) * 256
                if kind == "g":
                    P.op("act", lambda e, b=b, off=off, f4=f4: e.activation(out=sil4[:, f4, :], in_=bank(b)[:, off:off + 256], func=AF.Silu),
                         reads=(("ps", b),), writes=(("sil", f4),))
                else:
                    fc = q * 4 + f4
                    P.op("dve", lambda e, b=b, off=off, f4=f4, hi_=hi_, fc=fc: e.tensor_tensor(
                        out=hidT[hi_][:, fc, :], in0=sil4[:, f4, :], in1=bank(b)[:, off:off + 256], op=ALU.mult),
                        reads=(("sil", f4), ("ps", b)), writes=(("hidT", hi_),))

    def sel_build(ex):
        for tq in range(4):
            sti = 0
            for t4 in range(4):
                ti = tq * 4 + t4
                P.op("dve", lambda e, sti=sti, t4=t4, ti=ti, ex=ex: e.tensor_scalar(
                    out=selAT[sti][:, t4, :], in0=iota_f[:], scalar1=pos_tm[:, ti, ex:ex + 1], scalar2=A_tm[:, ti, ex:ex + 1],
                    op0=ALU.is_equal, op1=ALU.mult),
                    reads=("iota_f", "pos_tm", "A_tm"), writes=(("selAT", sti),))
            for sc in range(2):
                b = next_bank()
                for t4 in range(4):
                    P.op("pe", lambda e, b=b, t4=t4, sti=sti, sc=sc: e.transpose(bank_bf(b)[:, t4 * 128:(t4 + 1) * 128],
                                                                               selAT[sti][:, t4, sc * 128:(sc + 1) * 128], ident_bf[:]),
                         reads=(("selAT", sti), "ident_bf"), writes=(("ps", b),))
                evac(selA[:, sc, tq * 512:(tq + 1) * 512], bank_bf(b)[:, 0:512], reads=(("ps", b),), writes=("selA",))

    def down_step(ex, db, after_tile=None):
        _, _, wdv = wv(ex)
        hi_ = ex % 2
        sd = []
        for hf in range(2):
            s = next_slot()
            sd.append(s)
            P.dma("pool", "ws%d" % s, lambda e, s=s, hf=hf, db=db, wdv=wdv: e.dma_start(
                out=wd8[s][:], in_=wdv[:, hf * 8:(hf + 1) * 8, db * 512:(db + 1) * 512]), writes=(("ws", s),))
        for sc in range(2):
            b = next_bank()
            for fc in range(16):
                s = sd[fc // 8]
                mm(bank(b), hidT[hi_][:, fc, sc * 128:(sc + 1) * 128], wd8[s][:, fc % 8, :], fc == 0, fc == 15,
                   reads=(("ws", s), ("hidT", hi_)), writes=(("ps", b),))
            evac(ysb[:, sc, :], bank(b), reads=(("ps", b),), writes=(("ysb", sc),))
        for ti in range(16):
            b = next_bank()
            for sc in range(2):
                mm(bank(b), selA[:, sc, ti * 128:(ti + 1) * 128], ysb[:, sc, :], sc == 0, sc == 1,
                   reads=("selA", ("ysb", 0), ("ysb", 1)), writes=(("ps", b),))
            P.op("dve", lambda e, b=b, ti=ti, db=db: e.tensor_tensor(out=acc(ti)[:, db * 512:(db + 1) * 512],
                                                                    in0=acc(ti)[:, db * 512:(db + 1) * 512], in1=bank(b), op=ALU.add),
                 reads=(("ps", b), ("acc", ti)), writes=(("acc", ti),))
            if after_tile is not None:
                after_tile(ti)

    gfin = sb("gfin", [128, 2048], F32, 168.5)
    junk2 = sb("junk2", [128, 2048], BF16, 176.5)
    gfin_d = inp("gfin_rep", [128, 2048])
    out_d = nc.dram_tensor("out", [2048, 2048], F32, kind="ExternalOutput").ap()
    FSS, FINV = 0, 16

    def final_tile(ti):
        P.op("act", lambda e, ti=ti: e.activation(out=junk2[:], in_=acc(ti), func=AF.Square, accum_out=small[:, FSS + ti:FSS + ti + 1]),
             reads=(("acc", ti),), writes=("junk2", ("fss", ti), ("ssq", ti)))
        P.op("act", lambda e, ti=ti: e.activation(out=small[:, FINV + ti:FINV + ti + 1], in_=small[:, FSS + ti:FSS + ti + 1],
                                                 func=AF.Sqrt, bias=EPS, scale=1.0 / D),
             reads=(("fss", ti),), writes=(("finv", ti), ("inv2", ti)))
        P.op("dve", lambda e, ti=ti: e.reciprocal(out=small[:, FINV + ti:FINV + ti + 1], in_=small[:, FINV + ti:FINV + ti + 1]),
             reads=(("finv", ti),), writes=(("finv", ti),))
        P.op("dve", lambda e, ti=ti: e.scalar_tensor_tensor(out=acc(ti), in0=acc(ti), scalar=small[:, FINV + ti:FINV + ti + 1],
                                                           in1=gfin[:], op0=ALU.mult, op1=ALU.mult),
             reads=(("acc", ti), ("finv", ti), "gfin"), writes=(("acc", ti),))
        final_ops.append(P.dma("sp", "ost%d" % (ti % 4), lambda e, ti=ti: e.dma_start(out=out_d[ti * 128:(ti + 1) * 128, :], in_=acc(ti)),
                               reads=(("acc", ti),)))

    for ex in range(E + 1):
        if 0 < ex < E:
            xin_load(ex)
        if ex == E:
            P.alias(["gfin"], ["xinT"])
            P.alias(["junk2"], [("hidT", 0)])
            P.dma("sp", "gfin", lambda e: e.dma_start(out=gfin[:], in_=gfin_d), writes=("gfin",))
        for q in range(4):
            if ex < E:
                gate_up_step(ex, q)
            if ex >= 1:
                if q == 0:
                    sel_build(ex - 1)
                down_step(ex - 1, q, after_tile=final_tile if (ex == E and q == 3) else None)
    return finish()


def _consts():
    mats = np.zeros((6, 128, 128), np.float32)
    mats[0] = 1.0
    mats[1] = np.eye(128, dtype=np.float32)
    mats[2] = np.triu(np.ones((128, 128), np.float32), k=1)
    k = np.arange(128, dtype=np.float64)
    ang = 2 * np.pi * np.outer(k, k) / 128.0
    mats[3] = (np.cos(ang) / np.sqrt(128.0)).astype(np.float32)
    mats[4] = (-np.sin(ang) / np.sqrt(128.0)).astype(np.float32)
    iota = np.tile(np.arange(256, dtype=np.float32)[None, :], (128, 1))
    s = np.arange(S, dtype=np.int64)
    angS = 2 * np.pi * ((s[:, None] * s[None, :]) % S).astype(np.float64) / S
    cosS = (np.cos(angS) / np.sqrt(float(S))).astype(np.float32)
    sinS = (np.sin(angS) / np.sqrt(float(S))).astype(np.float32)
    invc = np.zeros((4, 128, S), np.float32)
    for g, w in enumerate((2, 4, 8, 16)):
        lo = np.clip(s - w // 2, 0, S)
        hi = np.clip(s + w - w // 2, 0, S)
        invc[g] = (1.0 / (hi - lo).astype(np.float64)).astype(np.float32)[None, :]
    return mats, iota, cosS, sinS, invc


def make_in_maps(inputs, in_shapes, cores):
    mats, iota, cosS, sinS, invc = _consts()
    f = lambda a: np.ascontiguousarray(np.asarray(a, dtype=np.float32))
    vec = np.zeros((128, 40), np.float32)
    vec[:, 0:16] = f(inputs["norm_mix_g"])[0].reshape(16, 128).T
    vec[:, 16:32] = f(inputs["norm_mem_g"])[0].reshape(16, 128).T
    vec[:, 32:40] = f(inputs["pool_scale"])[0].reshape(8, 128).T
    shared = {
        "cst_mats": mats, "cst_iota": iota, "cst_vecs": vec, "cst_cosS": cosS, "cst_sinS": sinS, "cst_invc": invc,
        "w_kv": f(inputs["w_kv_mem"])[0], "w_in": f(inputs["w_in"])[0],
        "pool_w": f(inputs["pool_w"])[0].reshape(1024, 256), "fourier_w": f(inputs["fourier_w"])[0].reshape(512, 128),
        "proj_pool": f(inputs["proj_pool"])[0], "proj_fourier": f(inputs["proj_fourier"])[0], "proj_mem": f(inputs["proj_mem"])[0],
        "w_out": f(inputs["w_out"])[0], "w_router": f(inputs["w_router"])[0],
        "gffn_rep": np.ascontiguousarray(np.tile(f(inputs["norm_ffn_g"])[0][None, :], (128, 1))),
        "gfin_rep": np.ascontiguousarray(np.tile(f(inputs["norm_final_g"])[None, :], (128, 1))),
        "w_gate": f(inputs["w_expert_gate"])[0], "w_up": f(inputs["w_expert_up"])[0], "w_down": f(inputs["w_expert_down"])[0],
    }
    x = f(inputs["x"])
    mem = f(inputs["mem"])
    maps = []
    for b in cores:
        m = {}
        for name in in_shapes:
            if name == "x":
                m[name] = x[b]
            elif name == "xT":
                m[name] = np.ascontiguousarray(x[b].T)
            elif name == "memT":
                m[name] = np.ascontiguousarray(mem[b].T)
            else:
                m[name] = shared[name]
            assert tuple(m[name].shape) == tuple(in_shapes[name]), (name, m[name].shape, in_shapes[name])
        maps.append(m)
    return maps


def kernel(**inputs):
    nc, in_shapes = build()
    maps = make_in_maps(inputs, in_shapes, list(range(8)))
    res = run_bass_kernel_spmd(nc, maps, core_ids=list(range(8)))
    return np.stack([np.asarray(r["out"], dtype=np.float32) for r in res.results], axis=0)
```
